# Optimizing a Trainium2 kernel written in Bass

```python
import math
import jax
import jax.numpy as jnp
from jax import lax
import numpy as np

D_MODEL = 1024
BATCH = 8
SEQ = 2048
DEPTH = 2

GRID_W = 64
CTX_LEN = 256
EPS = 1e-6
NEG_BIG = -1e30

A_HEADS = 4
A_DK = 128
A_DV = 128
A_CHUNK = 64
B_HEADS = 4
B_DK = 128
B_DV = 128
B_CHUNK = 64
B_CONV = 3
C_HEADS = 4
C_DH = 64
C_DV = 2 * C_DH
Q_BLOCK = 128
ROPE_BASE = 10000.0
ROPE_PAIRS = C_DH // 4

A_QK_W = A_HEADS * A_DK
A_V_W = A_HEADS * A_DV
B_QK_W = B_HEADS * B_DK
B_V_W = B_HEADS * B_DV
B_GATE_W = 4 * B_HEADS
C_QK_W = C_HEADS * 2 * C_DH
C_V_W = C_HEADS * C_DV
BRANCH_W = 512
PROJ_WIDTHS = (A_QK_W, A_V_W, A_V_W, A_QK_W, A_QK_W,
               B_QK_W, B_QK_W, B_V_W, B_V_W, B_GATE_W,
               C_QK_W, C_QK_W, C_V_W)

N_GROUPS = 4
EXPERTS_PER_GROUP = 8
N_EXPERTS = N_GROUPS * EXPERTS_PER_GROUP
TOP_K = 2
D_EXPERT = 512
MOE_BLOCK = 128

kernel_name = 'hybrid_hgrn2_mlstm_diffattn_hmoe_dit'


def _rms_norm(x, g):
    x32 = x.astype(jnp.float32)
    y = x32 * lax.rsqrt(jnp.mean(x32 * x32, axis=-1, keepdims=True) + EPS)
    return (y * g.astype(jnp.float32)).astype(x.dtype)


def _head_rms_norm(x, g, n_heads):
    shp = x.shape
    xh = x.reshape(shp[:-1] + (n_heads, -1)).astype(jnp.float32)
    y = xh * lax.rsqrt(jnp.mean(xh * xh, axis=-1, keepdims=True) + EPS)
    return (y.reshape(shp) * g.astype(jnp.float32)).astype(x.dtype)


def _heads(t, n_heads):
    return t.reshape(t.shape[:-1] + (n_heads, -1))


def _centred_dwconv(x, w, b):
    k = w.shape[0]
    pad = k // 2
    t = x.shape[1]
    xp = jnp.pad(x, ((0, 0), (pad, k - 1 - pad), (0, 0)))
    out = b
    for j in range(k):
        out = out + xp[:, j:j + t] * w[j]
    return out


def _rope_tables(n_tokens):
    rows = n_tokens // GRID_W
    row = jnp.repeat(jnp.arange(rows, dtype=jnp.float32), GRID_W)
    col = jnp.tile(jnp.arange(GRID_W, dtype=jnp.float32), rows)
    inv = ROPE_BASE ** (-jnp.arange(ROPE_PAIRS, dtype=jnp.float32) / ROPE_PAIRS)
    ang = jnp.stack([row[:, None] * inv, col[:, None] * inv])
    return jnp.cos(ang), jnp.sin(ang)


def _axial_rope(x, cos, sin):
    p = ROPE_PAIRS
    parts = []
    for a in range(2):
        seg = x[..., a * 2 * p:(a + 1) * 2 * p]
        x1, x2 = seg[..., :p], seg[..., p:]
        ca = cos[a][None, :, None, None, :].astype(x.dtype)
        sa = sin[a][None, :, None, None, :].astype(x.dtype)
        parts.append(x1 * ca - x2 * sa)
        parts.append(x2 * ca + x1 * sa)
    return jnp.concatenate(parts, axis=-1)


def _to_chunks(t, size):
    b, n, h = t.shape[:3]
    t = t.reshape((b, n // size, size, h) + t.shape[3:])
    return jnp.moveaxis(t, (1, 3), (0, 2))


def _from_chunks(t):
    t = jnp.moveaxis(t, (0, 2), (1, 3))
    return t.reshape((t.shape[0], t.shape[1] * t.shape[2]) + t.shape[3:])


def _hgrn2_scan(q, k, v, log_f, state0, with_out):
    xs = tuple(_to_chunks(t.astype(jnp.float32), A_CHUNK) for t in (q, k, v, log_f))
    tri = jnp.tril(jnp.ones((A_CHUNK, A_CHUNK), bool))[:, :, None]

    def step(s_prev, chunk):
        qc, kc, vc, lf = chunk
        b = jnp.cumsum(lf, axis=2)
        b_end = b[:, :, -1]
        s_new = jnp.exp(b_end)[..., None] * s_prev + jnp.einsum(
            'bhlk,bhlv->bhkv', kc * jnp.exp(b_end[:, :, None] - b), vc)
        if not with_out:
            return s_new, None
        rel = jnp.where(tri, jnp.exp(jnp.minimum(b[:, :, :, None] - b[:, :, None], 0.0)), 0.0)
        scores = jnp.einsum('bhtc,bhtsc,bhsc->bhts', qc, rel, kc)
        o = jnp.einsum('bhts,bhsv->bhtv', scores, vc) + jnp.einsum(
            'bhtk,bhkv->bhtv', qc * jnp.exp(b), s_prev)
        return s_new, o

    s_fin, o = lax.scan(step, state0, xs)
    if not with_out:
        return None, s_fin
    return _from_chunks(o).astype(v.dtype), s_fin


def _mlstm_scan(q, k, v, log_i, log_f, state0, with_out):
    xs = tuple(_to_chunks(t.astype(jnp.float32), B_CHUNK) for t in (q, k, v, log_i, log_f))
    tri = jnp.tril(jnp.ones((B_CHUNK, B_CHUNK), bool))

    def step(carry, chunk):
        c_prev, n_prev, m_prev = carry
        qc, kc, vc, li, lf = chunk
        b = jnp.cumsum(lf, axis=-1)
        b_end = b[..., -1]
        w_end = b_end[..., None] - b + li
        m_new = jnp.maximum(b_end + m_prev, jnp.max(w_end, axis=-1))
        e_end = jnp.exp(w_end - m_new[..., None])
        keep = jnp.exp(b_end + m_prev - m_new)
        c_new = keep[..., None, None] * c_prev + jnp.einsum('bhl,bhlk,bhlv->bhkv', e_end, kc, vc)
        n_new = keep[..., None] * n_prev + jnp.einsum('bhl,bhlk->bhk', e_end, kc)
        if not with_out:
            return (c_new, n_new, m_new), None
        logw = jnp.where(tri, b[..., :, None] - b[..., None, :] + li[..., None, :], NEG_BIG)
        w_state = b + m_prev[..., None]
        m_t = jnp.maximum(jnp.max(logw, axis=-1), w_state)
        s = jnp.einsum('bhtk,bhsk->bhts', qc, kc) * jnp.exp(logw - m_t[..., None])
        a_state = jnp.exp(w_state - m_t)
        num = jnp.einsum('bhts,bhsv->bhtv', s, vc) + a_state[..., None] * jnp.einsum(
            'bhtk,bhkv->bhtv', qc, c_prev)
        den = jnp.sum(s, axis=-1) + a_state * jnp.einsum('bhtk,bhk->bht', qc, n_prev)
        h = num / jnp.maximum(jnp.abs(den), jnp.exp(-m_t))[..., None]
        return (c_new, n_new, m_new), h

    state, h = lax.scan(step, state0, xs)
    if not with_out:
        return None, state
    return _from_chunks(h).astype(v.dtype), state


def _diff_attend(q, k, v, lam):
    s = jnp.einsum('bqhcd,bkhcd->bhcqk', q, k).astype(jnp.float32) * (C_DH ** -0.5)
    p = jax.nn.softmax(s, axis=-1)
    a = (p[:, :, 0] - lam * p[:, :, 1]).astype(v.dtype)
    return jnp.einsum('bhqk,bkhv->bqhv', a, v)


def _token_mixer(h_ctx, h_lat, w_in, conv_w, conv_b, gate_b, lb, a_g, b_g, c_g, lam_vec, lam_init,
                 w_branch, w_gate, b_gate, w_out, rope_cos, rope_sin, with_ctx):
    bsz, t_lat, _ = h_lat.shape
    t_ctx = h_ctx.shape[1]
    bounds = np.cumsum(PROJ_WIDTHS)[:-1].tolist()
    pc = jnp.split(h_ctx @ w_in, bounds, axis=-1)
    pl = jnp.split(h_lat @ w_in, bounds, axis=-1)
    directions = ((0, lambda t: t), (1, lambda t: t[:, ::-1]))

    def hgrn_gates(z, lb_d):
        z = _heads(z, A_HEADS).astype(jnp.float32)
        lb_h = lb_d.reshape(A_HEADS, A_DK)
        log_f = jnp.logaddexp(jnp.log(lb_h), jnp.log1p(-lb_h) + jax.nn.log_sigmoid(z))
        k = (1.0 - lb_h) * jax.nn.sigmoid(-z)
        return log_f, k

    qa_c, ia_c = _heads(pc[0], A_HEADS), _heads(pc[1], A_HEADS)
    qa_l, ia_l = _heads(pl[0], A_HEADS), _heads(pl[1], A_HEADS)
    a_lat, a_ctx = 0.0, 0.0
    for d, fl in directions:
        lf_c, k_c = hgrn_gates(pc[3 + d], lb[d])
        lf_l, k_l = hgrn_gates(pl[3 + d], lb[d])
        s0 = jnp.zeros((bsz, A_HEADS, A_DK, A_DV), jnp.float32)
        o_c, s_c = _hgrn2_scan(fl(qa_c), fl(k_c), fl(ia_c), fl(lf_c), s0, with_ctx)
        o_l, _ = _hgrn2_scan(fl(qa_l), fl(k_l), fl(ia_l), fl(lf_l), s_c, True)
        a_lat = a_lat + fl(o_l)
        if with_ctx:
            a_ctx = a_ctx + fl(o_c)
    ya_lat = _head_rms_norm(a_lat.reshape(bsz, t_lat, A_V_W), a_g, A_HEADS) * jax.nn.silu(pl[2])

    def mlstm_inputs(p):
        qk = jax.nn.silu(_centred_dwconv(jnp.concatenate([p[5], p[6]], axis=-1), conv_w, conv_b))
        q, k = jnp.split(qk, 2, axis=-1)
        q = _heads(q, B_HEADS) * (B_DK ** -0.5)
        g = p[9].astype(jnp.float32).reshape(p[9].shape[:-1] + (4, B_HEADS)) + gate_b
        return q, _heads(k, B_HEADS), _heads(p[7], B_HEADS), g

    qb_c, kb_c, vb_c, gb_c = mlstm_inputs(pc)
    qb_l, kb_l, vb_l, gb_l = mlstm_inputs(pl)
    b_lat, b_ctx = 0.0, 0.0
    for d, fl in directions:
        st0 = (jnp.zeros((bsz, B_HEADS, B_DK, B_DV), jnp.float32),
               jnp.zeros((bsz, B_HEADS, B_DK), jnp.float32),
               jnp.zeros((bsz, B_HEADS), jnp.float32))
        h_c, st_c = _mlstm_scan(fl(qb_c), fl(kb_c), fl(vb_c), fl(gb_c[..., d, :]),
                                fl(jax.nn.log_sigmoid(gb_c[..., 2 + d, :])), st0, with_ctx)
        h_l, _ = _mlstm_scan(fl(qb_l), fl(kb_l), fl(vb_l), fl(gb_l[..., d, :]),
                             fl(jax.nn.log_sigmoid(gb_l[..., 2 + d, :])), st_c, True)
        b_lat = b_lat + fl(h_l)
        if with_ctx:
            b_ctx = b_ctx + fl(h_c)
    yb_lat = _head_rms_norm(b_lat.reshape(bsz, t_lat, B_V_W), b_g, B_HEADS) * jax.nn.sigmoid(pl[8])

    lam_vec = lam_vec.astype(jnp.float32)
    lam = jnp.exp(jnp.sum(lam_vec[0] * lam_vec[1])) - jnp.exp(jnp.sum(lam_vec[2] * lam_vec[3])) + lam_init
    qc_c = pc[10].reshape(bsz, t_ctx, C_HEADS, 2, C_DH)
    kc_c = pc[11].reshape(bsz, t_ctx, C_HEADS, 2, C_DH)
    vc_c = pc[12].reshape(bsz, t_ctx, C_HEADS, C_DV)
    qc_l = _axial_rope(pl[10].reshape(bsz, t_lat, C_HEADS, 2, C_DH), rope_cos, rope_sin)
    kc_l = _axial_rope(pl[11].reshape(bsz, t_lat, C_HEADS, 2, C_DH), rope_cos, rope_sin)
    vc_l = pl[12].reshape(bsz, t_lat, C_HEADS, C_DV)
    k_all = jnp.concatenate([kc_c, kc_l], axis=1)
    v_all = jnp.concatenate([vc_c, vc_l], axis=1)
    n_blk = t_lat // Q_BLOCK
    q_blocks = jnp.moveaxis(qc_l.reshape(bsz, n_blk, Q_BLOCK, C_HEADS, 2, C_DH), 1, 0)
    o_l = lax.map(lambda qq: _diff_attend(qq, k_all, v_all, lam), q_blocks)
    o_l = jnp.moveaxis(o_l, 0, 1).reshape(bsz, t_lat, C_V_W)
    yc_lat = _head_rms_norm(o_l, c_g, C_HEADS) * (1.0 - lam_init)

    def merge(h, ya, yb, yc):
        ga, gb, gc = jnp.split(jax.nn.sigmoid(h @ w_gate + b_gate), 3, axis=-1)
        y = ga * (ya @ w_branch[0]) + gb * (yb @ w_branch[1]) + gc * (yc @ w_branch[2])
        return y @ w_out

    y_lat = merge(h_lat, ya_lat, yb_lat, yc_lat)
    if not with_ctx:
        return None, y_lat
    ya_ctx = _head_rms_norm(a_ctx.reshape(bsz, t_ctx, A_V_W), a_g, A_HEADS) * jax.nn.silu(pc[2])
    yb_ctx = _head_rms_norm(b_ctx.reshape(bsz, t_ctx, B_V_W), b_g, B_HEADS) * jax.nn.sigmoid(pc[8])
    o_c = _diff_attend(qc_c, kc_c, vc_c, lam).reshape(bsz, t_ctx, C_V_W)
    yc_ctx = _head_rms_norm(o_c, c_g, C_HEADS) * (1.0 - lam_init)
    return merge(h_ctx, ya_ctx, yb_ctx, yc_ctx), y_lat


def _hier_moe(tokens, rg_w, rg_b, re_w, re_b, w1, w3, w2):
    n_tok, d = tokens.shape
    group_logits = (tokens @ rg_w + rg_b).astype(jnp.float32)
    g_idx = jnp.argmax(group_logits, axis=-1)
    p_group = jnp.take_along_axis(jax.nn.softmax(group_logits, axis=-1), g_idx[:, None], axis=-1)
    exp_logits = (tokens @ re_w + re_b).astype(jnp.float32).reshape(n_tok, N_GROUPS, EXPERTS_PER_GROUP)
    in_group = jnp.take_along_axis(exp_logits, g_idx[:, None, None], axis=1)[:, 0]
    top_p, top_i = lax.top_k(jax.nn.softmax(in_group, axis=-1), TOP_K)
    weights = p_group * top_p / jnp.sum(top_p, axis=-1, keepdims=True)
    expert = g_idx[:, None] * EXPERTS_PER_GROUP + top_i

    n_assign = n_tok * TOP_K
    flat_e = expert.reshape(-1)
    flat_tok = jnp.repeat(jnp.arange(n_tok), TOP_K)
    flat_w = weights.reshape(-1)
    order = jnp.argsort(flat_e)
    e_s, tok_s, w_s = flat_e[order], flat_tok[order], flat_w[order]
    counts = jnp.bincount(flat_e, length=N_EXPERTS)
    start = jnp.cumsum(counts) - counts
    padded = (counts + MOE_BLOCK - 1) // MOE_BLOCK * MOE_BLOCK
    p_end = jnp.cumsum(padded)
    p_start = p_end - padded
    dest = p_start[e_s] + jnp.arange(n_assign) - start[e_s]
    n_blocks = -(-n_assign // MOE_BLOCK) + N_EXPERTS
    x_disp = jnp.zeros((n_blocks * MOE_BLOCK, d), tokens.dtype).at[dest].set(tokens[tok_s])
    block_e = jnp.minimum(jnp.searchsorted(p_end, jnp.arange(n_blocks) * MOE_BLOCK, side='right'),
                          N_EXPERTS - 1)

    def expert_block(args):
        xb, e = args
        return (jax.nn.silu(xb @ w1[e]) * (xb @ w3[e])) @ w2[e]

    y_disp = lax.map(expert_block, (x_disp.reshape(n_blocks, MOE_BLOCK, d), block_e)).reshape(-1, d)
    return jnp.zeros_like(tokens).at[tok_s].add(y_disp[dest] * w_s[:, None].astype(tokens.dtype))


def setup_inputs(seed: int = 0) -> dict:
    key = jax.random.key(seed)
    ks = jax.random.split(key, 32)
    f32 = jnp.float32
    dm = D_MODEL

    def nrm(k, shape, scale):
        return jax.random.normal(k, shape, f32) * scale

    proj_w = sum(PROJ_WIDTHS)
    gate_i = nrm(ks[12], (DEPTH, 2, B_HEADS), 0.1)
    gate_f = jnp.linspace(3.0, 6.0, B_HEADS, dtype=f32) + nrm(ks[13], (DEPTH, 2, B_HEADS), 0.1)
    return {
        'x': nrm(ks[0], (BATCH, SEQ, dm), 1.0),
        'c': nrm(ks[1], (BATCH, dm), 1.0),
        'ctx': nrm(ks[2], (BATCH, CTX_LEN, dm), 1.0),
        'c_ctx': nrm(ks[3], (dm,), 1.0),
        'ada_w': nrm(ks[4], (DEPTH, dm, 6 * dm), 0.5 * dm ** -0.5),
        'ada_b': nrm(ks[5], (DEPTH, 6 * dm), 0.02),
        'norm1_g': 1.0 + nrm(ks[6], (DEPTH, dm), 0.02),
        'norm2_g': 1.0 + nrm(ks[7], (DEPTH, dm), 0.02),
        'w_in': nrm(ks[8], (DEPTH, dm, proj_w), dm ** -0.5),
        'mlstm_conv_w': nrm(ks[9], (DEPTH, B_CONV, 2 * B_QK_W), B_CONV ** -0.5),
        'mlstm_conv_b': nrm(ks[10], (DEPTH, 2 * B_QK_W), 0.02),
        'mlstm_gate_b': jnp.concatenate([gate_i, gate_f], axis=1),
        'hgrn_lb_raw': nrm(ks[11], (DEPTH, 2, A_QK_W), 0.5),
        'hgrn_norm_g': 1.0 + nrm(ks[14], (DEPTH, A_V_W), 0.02),
        'mlstm_norm_g': 1.0 + nrm(ks[15], (DEPTH, B_V_W), 0.02),
        'diff_norm_g': 1.0 + nrm(ks[16], (DEPTH, C_V_W), 0.02),
        'diff_lambda': nrm(ks[17], (DEPTH, 4, C_DH), 0.1),
        'w_branch': nrm(ks[18], (DEPTH, 3, BRANCH_W, dm), BRANCH_W ** -0.5),
        'w_gate': nrm(ks[19], (DEPTH, dm, 3 * dm), dm ** -0.5),
        'b_gate': nrm(ks[20], (DEPTH, 3 * dm), 0.02),
        'w_out': nrm(ks[21], (DEPTH, dm, dm), dm ** -0.5),
        'router_g_w': nrm(ks[22], (DEPTH, dm, N_GROUPS), dm ** -0.5),
        'router_g_b': nrm(ks[23], (DEPTH, N_GROUPS), 0.01),
        'router_e_w': nrm(ks[24], (DEPTH, dm, N_EXPERTS), dm ** -0.5),
        'router_e_b': nrm(ks[25], (DEPTH, N_EXPERTS), 0.01),
        'moe_w1': nrm(ks[26], (DEPTH, N_EXPERTS, dm, D_EXPERT), dm ** -0.5),
        'moe_w3': nrm(ks[27], (DEPTH, N_EXPERTS, dm, D_EXPERT), dm ** -0.5),
        'moe_w2': nrm(ks[28], (DEPTH, N_EXPERTS, D_EXPERT, dm), D_EXPERT ** -0.5),
        'final_g': 1.0 + nrm(ks[29], (dm,), 0.02),
    }


def reference(x, c, ctx, c_ctx, ada_w, ada_b, norm1_g, norm2_g, w_in, mlstm_conv_w, mlstm_conv_b,
              mlstm_gate_b, hgrn_lb_raw, hgrn_norm_g, mlstm_norm_g, diff_norm_g, diff_lambda, w_branch,
              w_gate, b_gate, w_out, router_g_w, router_g_b, router_e_w, router_e_b, moe_w1, moe_w3,
              moe_w2, final_g):
    bsz, t_lat, dm = x.shape
    t_ctx = ctx.shape[1]
    rope_cos, rope_sin = _rope_tables(t_lat)
    lb_cum = jnp.cumsum(jax.nn.softmax(hgrn_lb_raw.astype(jnp.float32), axis=0), axis=0)
    lower_bounds = lb_cum - lb_cum[0]
    x_lat, x_ctx = x, ctx
    for l in range(DEPTH):
        with_ctx = l < DEPTH - 1
        lam_init = 0.8 - 0.6 * math.exp(-0.3 * l)
        mod_lat = jnp.split(jax.nn.silu(c) @ ada_w[l] + ada_b[l], 6, axis=-1)
        sh1, sc1, g1, sh2, sc2, g2 = [m[:, None, :] for m in mod_lat]
        csh1, csc1, cg1, csh2, csc2, cg2 = jnp.split(jax.nn.silu(c_ctx) @ ada_w[l] + ada_b[l], 6, axis=-1)

        h_lat = _rms_norm(x_lat, norm1_g[l]) * (1.0 + sc1) + sh1
        h_ctx = _rms_norm(x_ctx, norm1_g[l]) * (1.0 + csc1) + csh1
        y_ctx, y_lat = _token_mixer(h_ctx, h_lat, w_in[l], mlstm_conv_w[l], mlstm_conv_b[l], mlstm_gate_b[l],
                                    lower_bounds[l], hgrn_norm_g[l], mlstm_norm_g[l], diff_norm_g[l],
                                    diff_lambda[l], lam_init, w_branch[l], w_gate[l], b_gate[l], w_out[l],
                                    rope_cos, rope_sin, with_ctx)
        x_lat = x_lat + g1 * y_lat
        h_lat = _rms_norm(x_lat, norm2_g[l]) * (1.0 + sc2) + sh2
        moe_params = (router_g_w[l], router_g_b[l], router_e_w[l], router_e_b[l],
                      moe_w1[l], moe_w3[l], moe_w2[l])
        if with_ctx:
            x_ctx = x_ctx + cg1 * y_ctx
            h_ctx = _rms_norm(x_ctx, norm2_g[l]) * (1.0 + csc2) + csh2
            tokens = jnp.concatenate([h_ctx.reshape(-1, dm), h_lat.reshape(-1, dm)], axis=0)
            y = _hier_moe(tokens, *moe_params)
            x_ctx = x_ctx + cg2 * y[:bsz * t_ctx].reshape(bsz, t_ctx, dm)
            x_lat = x_lat + g2 * y[bsz * t_ctx:].reshape(bsz, t_lat, dm)
        else:
            x_lat = x_lat + g2 * _hier_moe(h_lat.reshape(-1, dm), *moe_params).reshape(bsz, t_lat, dm)
    return _rms_norm(x_lat, final_g)
```

```python
import contextlib
import numpy as np
import concourse.bass as bass
import concourse.mybir as mybir
from concourse.bass_utils import run_bass_kernel_spmd

F32 = mybir.dt.float32
BF16 = mybir.dt.bfloat16
I32 = mybir.dt.int32
AF = mybir.ActivationFunctionType
ALU = mybir.AluOpType
AX = mybir.AxisListType


class Buf:
    __slots__ = ("w", "r", "name")

    def __init__(self, name=""):
        self.w = None
        self.r = {}
        self.name = name


class Prog:
    ENG = ("pe", "act", "dve", "pool", "sp")

    def __init__(self, nc, ndma=12):
        self.nc = nc
        self.st = {e: [] for e in self.ENG}
        self.cnt = {e: 0 for e in self.ENG}
        self.seen = {e: {} for e in self.ENG}
        self.es = contextlib.ExitStack()
        self.sem = {}
        for e in ("pe", "act", "dve", "pool"):
            self.sem[("c", e)] = self.es.enter_context(nc.semaphore("s_" + e))
        self.ndma = ndma
        self.dcnt = {}
        self.dnext = {}
        for q in ("sp", "pool"):
            self.dnext[q] = 0
            for i in range(ndma):
                self.sem[("d", q, i)] = self.es.enter_context(nc.semaphore("d_%s%d" % (q, i)))
                self.dcnt[(q, i)] = 0
        self.nops = 0

    def sb(self, name, shape, dt, stack=None):
        self.uid = getattr(self, "uid", 0) + 1
        return (stack or self.es).enter_context(self.nc.sbuf_tensor("%s_%d" % (name, self.uid), list(shape), dt))

    def ps(self, name, shape, dt=F32, stack=None):
        self.uid = getattr(self, "uid", 0) + 1
        return (stack or self.es).enter_context(self.nc.psum_tensor("%s_%d" % (name, self.uid), list(shape), dt))

    def op(self, eng, fn, reads=(), writes=(), dma=False):
        deps = {}

        def add(tok):
            if tok is None:
                return
            k, v = tok
            if deps.get(k, 0) < v:
                deps[k] = v

        for b in reads:
            add(b.w)
        for b in writes:
            add(b.w)
            for k, v in b.r.items():
                add((k, v))
        if dma:
            q = eng
            i = self.dnext[q]
            self.dnext[q] = (i + 1) % self.ndma
            k = self.dcnt[(q, i)]
            if k > 0:
                add((("d", q, i), 16 * k))
            self.dcnt[(q, i)] = k + 1
            token = (("d", q, i), 16 * (k + 1))
        else:
            self.cnt[eng] += 1
            token = (("c", eng), self.cnt[eng])
        waits = []
        seen = self.seen[eng]
        for k, v in deps.items():
            if k == ("c", "pe") and eng == "pe":
                continue
            if seen.get(k, 0) >= v:
                continue
            seen[k] = v
            waits.append((k, v))
        self.st[eng].append((waits, fn, token, dma))
        for b in writes:
            b.w = token
            b.r = {}
        for b in reads:
            if b.r.get(token[0], 0) < token[1]:
                b.r[token[0]] = token[1]
        self.nops += 1
        return token

    def barrier(self):
        toks = []
        for e in ("pe", "act", "dve", "pool"):
            if self.cnt[e] > 0:
                toks.append((("c", e), self.cnt[e]))
        for q in ("sp", "pool"):
            for i in range(self.ndma):
                k = self.dcnt[(q, i)]
                if k > 0:
                    toks.append((("d", q, i), 16 * k))
        for e in self.ENG:
            waits = []
            seen = self.seen[e]
            for k, v in toks:
                if seen.get(k, 0) >= v:
                    continue
                seen[k] = v
                waits.append((k, v))
            if waits:
                self.st[e].append((waits, None, None, False))
        self.flush()

    def emit(self):
        self.barrier()

    def flush(self):
        if not any(self.st[e] for e in self.ENG):
            return
        nc = self.nc
        sem = self.sem
        st = self.st

        def run(e, eng):
            for waits, fn, token, dma in st[e]:
                for k, v in waits:
                    eng.wait_ge(sem[k], v)
                if fn is None:
                    continue
                ins = fn(eng)
                ins.then_inc(sem[token[0]], 16 if dma else 1)

        with nc.Block() as block:
            @block.tensor
            def _(eng):
                run("pe", eng)

            @block.scalar
            def _(eng):
                run("act", eng)

            @block.vector
            def _(eng):
                run("dve", eng)

            @block.gpsimd
            def _(eng):
                run("pool", eng)

            @block.sync
            def _(eng):
                run("sp", eng)
        self.st = {e: [] for e in self.ENG}

    def dma(self, out, in_, reads=(), writes=(), q="sp", **kw):
        return self.op(q, lambda e: e.dma_start(out=out, in_=in_, **kw), reads, writes, dma=True)

    def mm(self, out, lhsT, rhs, start, stop, reads=(), writes=(), **kw):
        return self.op("pe", lambda e: e.matmul(out, lhsT, rhs, start=start, stop=stop, **kw), reads, writes)

    def tr(self, out, in_, ident, reads=(), writes=()):
        return self.op("pe", lambda e: e.transpose(out, in_, ident), reads, writes)

    def act(self, out, in_, func, reads=(), writes=(), **kw):
        return self.op("act", lambda e: e.activation(out, in_, func, **kw), reads, writes)


def _sugar():
    def tt(self, eng, out, in0, in1, op, reads=(), writes=()):
        return self.op(eng, lambda e: e.tensor_tensor(out, in0, in1, op), reads, writes)

    def ts(self, eng, out, in0, s1, s2, op0, op1=None, reads=(), writes=(), **kw):
        if op1 is None:
            return self.op(eng, lambda e: e.tensor_scalar(out, in0, s1, None, op0, **kw), reads, writes)
        return self.op(eng, lambda e: e.tensor_scalar(out, in0, s1, s2, op0, op1, **kw), reads, writes)

    def stt(self, out, in0, scalar, in1, op0, op1, reads=(), writes=()):
        return self.op("dve", lambda e: e.scalar_tensor_tensor(out, in0, scalar, in1, op0, op1), reads, writes)

    def copy(self, eng, out, in_, reads=(), writes=()):
        if eng == "act":
            return self.op("act", lambda e: e.copy(out, in_), reads, writes)
        return self.op(eng, lambda e: e.tensor_copy(out, in_), reads, writes)

    def memset(self, eng, ap, val, writes=()):
        return self.op(eng, lambda e: e.memset(ap, val), (), writes)

    def recip(self, out, in_, reads=(), writes=()):
        return self.op("dve", lambda e: e.reciprocal(out, in_), reads, writes)

    def reduce(self, out, in_, op, reads=(), writes=(), axis=AX.X):
        return self.op("dve", lambda e: e.tensor_reduce(out, in_, axis, op), reads, writes)

    for f in (tt, ts, stt, copy, memset, recip, reduce):
        setattr(Prog, f.__name__, f)


_sugar()
import math
NT = 2304
NCTX = 256
NLAT = 2048
D = 1024
BLK = [(0, 256), (256, 512), (768, 512), (1280, 512), (1792, 512)]
EPS = 1e-6
CO = dict(Aq=0, Ai=512, Ag=1024, Aff=1536, Afb=2048, Bq=2560, Bk=3072, Bv=3584, Bo=4096, Bg=4608, Cq=4624, Ck=5136, Cv=5648)
FM = ["Aq", "Ag", "Aff", "Afb", "Bq", "Bk", "Bo", "Cq", "Ck"]
FMI = {n: 4 * i for i, n in enumerate(FM)}
TM = [("Ai", 0, 512), ("Bv", 512, 512), ("Cv", 1024, 512), ("Bg", 1536, 16)]
TMO = {n: o for n, o, w in TM}
NTM = 1552


class Ring:
    def __init__(self, items):
        self.items = items
        self.i = 0

    def next(self):
        x = self.items[self.i % len(self.items)]
        self.i += 1
        return x


def host_consts():
    c = {}
    c["ident"] = np.eye(128, dtype=np.float32)
    c["ones"] = np.ones((128, 128), np.float32)
    s = np.arange(128)[:, None]
    t = np.arange(128)[None, :]
    c["tri_f"] = (t >= s).astype(np.float32)
    c["tri_b"] = (t <= s).astype(np.float32)
    c["tri_fs"] = (t > s).astype(np.float32)
    s6 = np.arange(64)[:, None]; t6 = np.arange(64)[None, :]
    c["tri_bsw"] = ((((s6 + 32) % 64) >= t6)).astype(np.float32)
    tl = np.arange(NLAT)
    row = (tl // 64).astype(np.float32)
    col = (tl % 64).astype(np.float32)
    inv = (10000.0 ** (-np.arange(16, dtype=np.float32) / 16)).astype(np.float32)
    cosT = np.zeros((128, NLAT), np.float32)
    sinT = np.zeros((128, NLAT), np.float32)
    pm = np.zeros((128, 128), np.float32)
    for p in range(128):
        j = p % 32
        a = (p % 64) // 32
        i = j % 16
        pos = row if a == 0 else col
        ang = (pos * inv[i]).astype(np.float32)
        cosT[p] = np.cos(ang)
        sinT[p] = np.sin(ang)
        if j < 16:
            pm[p + 16, p] = -1.0
        else:
            pm[p - 16, p] = 1.0
    c["cosT"] = cosT
    c["sinT"] = sinT
    c["pm"] = pm
    sel = np.zeros((32, 32, 128), np.float32)
    for e in range(32):
        sel[e, e, :] = 1.0
    c["sel"] = sel.reshape(32, 32 * 128)
    m = np.ones((128, NT), np.float32)
    m[:, ::64] = 0.0
    c["rst64"] = m
    return c


CONST_SHAPES = dict(ident=[128, 128], ones=[128, 128], tri_f=[128, 128], tri_b=[128, 128], tri_fs=[128, 128], tri_bsw=[64, 64],
                    cosT=[128, NLAT], sinT=[128, NLAT], pm=[128, 128], sel=[32, 32 * 128], rst64=[128, NT])

IN_SHAPES = dict(
    x=[NLAT, D], ctx=[NCTX, D], c=[D], c_ctx=[D], ada_w=[2, D, 6 * D], ada_b=[2, 6 * D], norm1_g=[2, D], norm2_g=[2, D],
    w_in=[2, D, 6160], mlstm_conv_w=[2, 3, 1024], mlstm_conv_b=[2, 1024], mlstm_gate_b=[2, 16], hgrn_lb_raw=[2, 2, 512],
    hgrn_norm_g=[2, 512], mlstm_norm_g=[2, 512], diff_norm_g=[2, 512], diff_lambda=[2, 256], w_branch=[2, 3, 512, D],
    w_gate=[2, D, 3 * D], b_gate=[2, 3 * D], w_out=[2, D, D], router_g_w=[2, D, 4], router_g_b=[2, 4], router_e_w=[2, D, 32],
    router_e_b=[2, 32], moe_w1=[2, 32, D, 512], moe_w3=[2, 32, D, 512], moe_w2=[2, 32, 512, D], final_g=[D])


def build(stop_after=None, debug=False, layers=2):
    nc = bass.Bass("TRN2", target_bir_lowering=False)
    I = {k: nc.dram_tensor(k, s, F32, kind="ExternalInput").ap() for k, s in IN_SHAPES.items()}
    C = {k: nc.dram_tensor("k_" + k, s, F32, kind="ExternalInput").ap() for k, s in CONST_SHAPES.items()}
    OUT = nc.dram_tensor("out", [NLAT, D], F32, kind="ExternalOutput").ap()
    dk = "ExternalOutput" if debug else "Internal"
    XT_d = nc.dram_tensor("XT_d", [128, 8, NT], F32, kind=dk).ap()
    HT_d = nc.dram_tensor("HT_d", [128, 8, NT], BF16, kind=dk).ap()
    PF_d = nc.dram_tensor("PF_d", [36, 128, NT], F32, kind=dk).ap()
    PT_d = nc.dram_tensor("PT_d", [NT, NTM], F32, kind=dk).ap()
    Y_d = nc.dram_tensor("Y_d", [3, 128, 4, NT], BF16, kind=dk).ap()
    bXT, bHT, bPF, bPT, bY = Buf(), Buf(), [Buf() for _ in range(36)], Buf(), [Buf() for _ in range(3)]
    P = Prog(nc)
    ES = contextlib.ExitStack

    ident = P.sb("ident", [128, 128], F32); ones = P.sb("ones", [128, 128], F32)
    identb = P.sb("identb", [128, 128], BF16); onesb = P.sb("onesb", [128, 128], BF16)
    epsT = P.sb("epsT", [128, 1], F32)
    bconst = Buf()
    P.dma(ident[:], C["ident"], writes=[bconst]); P.dma(ones[:], C["ones"], writes=[bconst])
    P.copy("dve", identb[:], ident[:], [bconst], [bconst]); P.copy("dve", onesb[:], ones[:], [bconst], [bconst])
    P.memset("dve", epsT[:], EPS, [bconst])
    MOD = P.sb("MOD", [128, 48, 2], F32); bMOD = Buf()
    SC = P.sb("SC", [128, 2, 8, 2], F32)
    bSC = Buf()

    def vec_fm(dst, src, **kw):
        return P.dma(dst, src.rearrange("(k p) -> p k", p=128), allow_slow_non_contiguous=True, **kw)

    MODs = [MOD] + [P.sb("MOD%d" % i, [128, 48, 2], F32) for i in range(1, layers)]
    SCs = [SC] + [P.sb("SC%d" % i, [128, 2, 8, 2], F32) for i in range(1, layers)]
    with ES() as ph:
        stg = [P.sb("Lstg%d" % i, [128, D], F32, ph) for i in range(3)]; bstg = [Buf() for _ in range(3)]
        xo = [P.sb("Lxo%d" % i, [128, 8, 128], F32, ph) for i in range(2)]; bxo = [Buf() for _ in range(2)]
        pst = [P.ps("Lps%d" % i, [128, 512], F32, ph) for i in range(4)]; bps = [Buf() for _ in range(4)]
        cs = P.sb("Mcs", [128, 8, 2], F32, ph); bcs = Buf()
        vec_fm(cs[:, :, 0], I["c"], writes=[bcs]); vec_fm(cs[:, :, 1], I["c_ctx"], writes=[bcs])
        P.act(cs[:], cs[:], AF.Silu, [bcs], [bcs])
        ab = P.sb("Mab", [128, layers, 48], F32, ph); bab = Buf()
        ng = P.sb("Mng", [128, layers, 2, 8], F32, ph)
        for l_ in range(layers):
            vec_fm(ab[:, l_], I["ada_b"][l_], writes=[bab])
            vec_fm(ng[:, l_, 0, :], I["norm1_g"][l_], writes=[bab]); vec_fm(ng[:, l_, 1, :], I["norm2_g"][l_], writes=[bab])
        wst = [P.sb("Mw%d" % i, [128, 8, 768], F32, ph) for i in range(4)]; bw = [Buf() for _ in range(4)]
        pm_ = [P.ps("Mps%d" % i, [128, 512], F32, ph) for i in range(layers)]; bpm = [Buf() for _ in range(layers)]

        def L_step(t):
            src = I["ctx"][t * 128:(t + 1) * 128, :] if t < 2 else I["x"][(t - 2) * 128:(t - 1) * 128, :]
            s_ = t % 3
            P.dma(stg[s_][:], src, writes=[bstg[s_]], q="sp" if t % 2 == 0 else "pool")
            o = t % 2
            for half in range(2):
                pi = (2 * t + half) % 4
                for j in range(4):
                    kc = half * 4 + j
                    P.tr(pst[pi][:, j * 128:(j + 1) * 128], stg[s_][:, kc * 128:(kc + 1) * 128], ident[:], [bstg[s_], bconst], [bps[pi]])
                P.copy("dve" if half == 0 else "act", xo[o][:, half * 4:half * 4 + 4, :], pst[pi][:].rearrange("p (j c) -> p j c", j=4), [bps[pi]], [bxo[o]])
            P.dma(XT_d[:, :, t * 128:(t + 1) * 128], xo[o][:], reads=[bxo[o]], writes=[bXT])

        def M_group(l_, gI):
            k_ = (l_ * 8 + gI) % 4
            w = wst[k_]
            P.dma(w[:], I["ada_w"][l_, :, gI * 768:(gI + 1) * 768].rearrange("(k p) c -> p k c", p=128), writes=[bw[k_]], q="pool" if gI % 2 == 0 else "sp")
            for jj in range(6):
                j = gI * 6 + jj
                for kc in range(8):
                    P.mm(pm_[l_][:, 2 * j:2 * j + 2], w[:, kc, jj * 128:(jj + 1) * 128], cs[:, kc, :], kc == 0, kc == 7, [bw[k_], bcs], [bpm[l_]])

        def M_final(l_):
            MOD_ = MODs[l_]; SC_ = SCs[l_]
            P.tt("dve", MOD_[:], pm_[l_][:, 0:96].rearrange("p (j w) -> p j w", w=2), ab[:, l_].unsqueeze(2).to_broadcast([128, 48, 2]), ALU.add, [bpm[l_], bab], [bMOD])
            for n, sect in ((0, 1), (1, 4)):
                P.ts("dve", SC_[:, n], MOD_[:, sect * 8:(sect + 1) * 8, :], 1.0, None, ALU.add, reads=[bMOD], writes=[bSC])
                P.tt("dve", SC_[:, n], SC_[:, n], ng[:, l_, n, :].unsqueeze(2).to_broadcast([128, 8, 2]), ALU.mult, [bSC, bab], [bSC])

        mi = 0
        for t in range(18):
            L_step(t)
            if mi < layers * 8:
                M_group(mi // 8, mi % 8)
                if mi % 8 == 7:
                    M_final(mi // 8)
                mi += 1
        while mi < layers * 8:
            M_group(mi // 8, mi % 8)
            if mi % 8 == 7:
                M_final(mi // 8)
            mi += 1
        P.barrier()
    if stop_after == "L":
        P.emit(); return nc

    def final_phase(xres, bxres):
      with ES() as ph:
        fg = P.sb("Ffg", [128, 8], F32, ph); bfg = Buf()
        vec_fm(fg[:], I["final_g"], writes=[bfg])
        sq = P.sb("Fsq", [128, 8, 512], F32, ph); bsq = Buf()
        rs = P.sb("Frs", [128, 512], F32, ph); brs = Buf()
        ob = [P.sb("Fo%d" % i, [128, 1024], F32, ph) for i in range(2)]; bob = [Buf(), Buf()]
        pn = P.ps("Fpn", [128, 512], F32, ph); bpn = Buf()
        pt = [P.ps("Fpt%d" % i, [128, 512], F32, ph) for i in range(4)]; bpt = [Buf() for _ in range(4)]
        ti = 0
        for bi, (b0, bs) in enumerate(BLK[1:]):
            xv = xres[:, :, b0:b0 + bs]
            P.act(sq[:], xv, AF.Square, [bxres], [bsq])
            for kc in range(8):
                P.mm(pn[:], ones[:], sq[:, kc, :], kc == 0, kc == 7, [bsq, bconst], [bpn])
            P.act(rs[:], pn[:], AF.Sqrt, [bpn, bconst], [brs], bias=epsT[:], scale=1.0 / D)
            P.recip(rs[:], rs[:], [brs], [brs])
            P.tt("dve", sq[:], xv, rs[:].unsqueeze(1).to_broadcast([128, 8, 512]), ALU.mult, [bxres, brs], [bsq])
            for kc in range(8):
                P.act(sq[:, kc, :], sq[:, kc, :], AF.Copy, [bsq, bfg], [bsq], scale=fg[:, kc:kc + 1])
            for tt_ in range(4):
                o2 = ti % 2; ti += 1
                for half in range(2):
                    p4 = (2 * ti + half) % 4
                    for j in range(4):
                        kc = half * 4 + j
                        P.tr(pt[p4][:, j * 128:(j + 1) * 128], sq[:, kc, tt_ * 128:(tt_ + 1) * 128], ident[:], [bsq, bconst], [bpt[p4]])
                    P.copy("dve" if half == 0 else "act", ob[o2][:, half * 512:(half + 1) * 512], pt[p4][:], [bpt[p4]], [bob[o2]])
                r0 = b0 - NCTX + tt_ * 128
                P.dma(OUT[r0:r0 + 128, :], ob[o2][:], reads=[bob[o2]], q="sp" if tt_ % 2 else "pool")
        P.barrier()

    for l in range(layers):
        last = (l == 1)
        MOD = MODs[l]; SC = SCs[l]

        def norm_phase(n, blocks, router=None, into=None):
            shs = 0 if n == 0 else 3
            with ES() as ph_own:
                ph = ph_own if into is None else into[2]
                nsq = 2 if into is None else 1
                xb = [P.sb("Nx%d" % i, [128, 8, 512], F32, ph) for i in range(2)]; bxb = [Buf(), Buf()]
                sq_ = [P.sb("Nsq%d" % i, [128, 8, 512], F32, ph) for i in range(nsq)]; bsq_ = [Buf() for _ in range(nsq)]
                rs_ = [P.sb("Nrs%d" % i, [128, 512], F32, ph) for i in range(2)]; brs_ = [Buf(), Buf()]
                if into is None:
                    hb = [P.sb("Nh%d" % i, [128, 8, 512], BF16, ph) for i in range(2)]; bhb = [Buf(), Buf()]
                pn = [P.ps("Nps%d" % i, [128, 512], F32, ph) for i in range(2)]; bpn = [Buf(), Buf()]
                for bi, (b0, bs) in enumerate(blocks):
                    if into is not None:
                        i2 = bi % 2
                        sq = sq_[0]; bsq = bsq_[0]; rs = rs_[i2]; brs = brs_[i2]
                        who = 1 if b0 < NCTX else 0
                        hT_i, bh_i = into[0], into[1][bi]
                        P.dma(xb[i2][:, :, :bs], XT_d[:, :, b0:b0 + bs], reads=[bXT], writes=[bxb[i2]], q="sp" if i2 == 0 else "pool")
                        P.act(sq[:, :, :bs], xb[i2][:, :, :bs], AF.Square, [bxb[i2]], [bsq])
                        for kc in range(8):
                            P.mm(pn[i2][:, :bs], ones[:], sq[:, kc, :bs], kc == 0, kc == 7, [bsq, bconst], [bpn[i2]])
                        P.act(rs[:, :bs], pn[i2][:, :bs], AF.Sqrt, [bpn[i2], bconst], [brs], bias=epsT[:], scale=1.0 / D)
                        P.recip(rs[:, :bs], rs[:, :bs], [brs], [brs])
                        P.tt("dve", sq[:, :, :bs], xb[i2][:, :, :bs], rs[:, :bs].unsqueeze(1).to_broadcast([128, 8, bs]), ALU.mult, [bxb[i2], brs], [bsq])
                        for kc in range(8):
                            P.act(hT_i[:, kc, b0:b0 + bs], sq[:, kc, :bs], AF.Identity, [bsq, bSC, bMOD], [bh_i],
                                  scale=SC[:, n, kc, who:who + 1], bias=MOD[:, shs * 8 + kc, who:who + 1])
                        P.dma(HT_d[:, :, b0:b0 + bs], hT_i[:, :, b0:b0 + bs], reads=[bh_i], writes=[bHT], q="pool")
                        continue
                    who = 1 if b0 < NCTX else 0
                    i2 = bi % 2
                    sq = sq_[i2]; bsq = bsq_[i2]; rs = rs_[i2]; brs = brs_[i2]
                    P.dma(xb[i2][:, :, :bs], XT_d[:, :, b0:b0 + bs], reads=[bXT], writes=[bxb[i2]], q="sp" if i2 == 0 else "pool")
                    P.act(sq[:, :, :bs], xb[i2][:, :, :bs], AF.Square, [bxb[i2]], [bsq])
                    for kc in range(8):
                        P.mm(pn[i2][:, :bs], ones[:], sq[:, kc, :bs], kc == 0, kc == 7, [bsq, bconst], [bpn[i2]])
                    P.act(rs[:, :bs], pn[i2][:, :bs], AF.Sqrt, [bpn[i2], bconst], [brs], bias=epsT[:], scale=1.0 / D)
                    P.recip(rs[:, :bs], rs[:, :bs], [brs], [brs])
                    P.tt("dve", sq[:, :, :bs], xb[i2][:, :, :bs], rs[:, :bs].unsqueeze(1).to_broadcast([128, 8, bs]), ALU.mult, [bxb[i2], brs], [bsq])
                    if router is None:
                        for kc in range(8):
                            P.act(hb[i2][:, kc, :bs], sq[:, kc, :bs], AF.Identity, [bsq, bSC, bMOD], [bhb[i2]],
                                  scale=SC[:, n, kc, who:who + 1], bias=MOD[:, shs * 8 + kc, who:who + 1])
                    else:
                        rw_, LG_, bLG_, brw_ = router
                        for kc in range(8):
                            P.act(sq[:, kc, :bs], sq[:, kc, :bs], AF.Identity, [bsq, bSC, bMOD], [bsq],
                                  scale=SC[:, n, kc, who:who + 1], bias=MOD[:, shs * 8 + kc, who:who + 1])
                        P.copy("act", hb[i2][:, 0:4, :bs], sq[:, 0:4, :bs], [bsq], [bhb[i2]])
                        P.copy("dve", hb[i2][:, 4:8, :bs], sq[:, 4:8, :bs], [bsq], [bhb[i2]])
                        for tt_ in range(bs // 128):
                            tg = b0 // 128 + tt_
                            for kc in range(8):
                                P.mm(pn[i2][:, 0:36], sq[:, kc, tt_ * 128:(tt_ + 1) * 128], rw_[:, kc, :], kc == 0, kc == 7, [bsq, brw_], [bpn[i2]])
                            P.copy("dve", LG_[:, tg, :], pn[i2][:, 0:36], [bpn[i2]], [bLG_])
                    P.dma(HT_d[:, :, b0:b0 + bs], hb[i2][:, :, :bs], reads=[bhb[i2]], writes=[bHT])
                if into is None:
                    P.barrier()

        if stop_after == "N1":
            norm_phase(0, BLK)
            P.emit(); return nc

        with ES() as ph:
            hT = P.sb("PhT", [128, 8, NT], BF16, ph); bhs = [Buf() for _ in BLK]
            norm_phase(0, BLK, into=(hT, bhs, ph))

            def bh_of(c0, c1):
                return [bhs[i] for i, (b0, bs) in enumerate(BLK) if b0 < c1 and c0 < b0 + bs]
            wst = [P.sb("Pws%d" % i, [128, 8, 512], F32, ph) for i in range(2)]; bws = [Buf(), Buf()]
            wb = [P.sb("Pwb%d" % i, [128, 8, 512], BF16, ph) for i in range(2)]; bwb = [Buf(), Buf()]
            og = [P.sb("Pog%d" % i, [128, NT], F32, ph) for i in range(2)]; bog = [Buf(), Buf()]
            ot = [P.sb("Pot%d" % i, [128, 512], F32, ph) for i in range(3)]; bot = [Buf() for _ in range(3)]
            pp = [P.ps("Pps%d" % i, [128, 512], F32, ph) for i in range(4)]; bpp = [Buf() for _ in range(4)]
            gi = 0; ei = 0; oi = 0; ti = 0

            def loadw(c0, wdt):
                nonlocal gi
                i2 = gi % 2; gi += 1
                P.dma(wst[i2][:, :, :wdt], I["w_in"][l, :, c0:c0 + wdt].rearrange("(k p) c -> p k c", p=128), writes=[bws[i2]], q="sp" if i2 == 0 else "pool")
                P.copy("act" if i2 == 0 else "dve", wb[i2][:, :, :wdt], wst[i2][:, :, :wdt], [bws[i2]], [bwb[i2]])
                return wb[i2], bwb[i2]

            for name in FM:
                w, bw_ = loadw(CO[name], 512)
                for ch in range(4):
                    o2 = oi % 2; oi += 1
                    for (b0, bs) in BLK:
                        pi = ei % 4; ei += 1
                        for kc in range(8):
                            P.mm(pp[pi][:, :bs], w[:, kc, ch * 128:(ch + 1) * 128], hT[:, kc, b0:b0 + bs], kc == 0, kc == 7, [bw_] + bh_of(b0, b0 + bs), [bpp[pi]])
                        P.copy("act" if ei % 2 == 0 else "dve", og[o2][:, b0:b0 + bs], pp[pi][:, :bs], [bpp[pi]], [bog[o2]])
                    P.dma(PF_d[FMI[name] + ch], og[o2][:], reads=[bog[o2]], writes=[bPF[FMI[name] + ch]])
            for name, o0, wdt in TM:
                w, bw_ = loadw(CO[name], wdt)
                for t in range(18):
                    pi = ei % 4; ei += 1
                    for kc in range(8):
                        P.mm(pp[pi][:, :wdt], hT[:, kc, t * 128:(t + 1) * 128], w[:, kc, :wdt], kc == 0, kc == 7, [bw_] + bh_of(t * 128, (t + 1) * 128), [bpp[pi]])
                    t3 = ti % 3; ti += 1
                    P.copy("act" if ei % 2 == 0 else "dve", ot[t3][:, :wdt], pp[pi][:, :wdt], [bpp[pi]], [bot[t3]])
                    P.dma(PT_d[t * 128:(t + 1) * 128, o0:o0 + wdt], ot[t3][:, :wdt], reads=[bot[t3]], writes=[bPT], q="pool" if t % 2 else "sp")
            P.barrier()
        if stop_after == "P":
            P.emit(); return nc

        lam_init = 0.8 - 0.6 * math.exp(-0.3 * l)
        qblocks = (BLK if not last else BLK[1:])
        with ES() as ph:
            cosT = P.sb("Ccos", [128, NLAT], F32, ph); sinT = P.sb("Csin", [128, NLAT], F32, ph); pm = P.sb("Cpm", [128, 128], F32, ph)
            bcc = Buf()
            P.dma(cosT[:], C["cosT"], writes=[bcc]); P.dma(sinT[:], C["sinT"], writes=[bcc], q="pool"); P.dma(pm[:], C["pm"], writes=[bcc])
            lv = P.sb("Clv", [128, 256], F32, ph); lsc = P.sb("Clsc", [128, 4], F32, ph); nlam = P.sb("Cnlam", [128, 1], F32, ph); blam = Buf()
            P.dma(lv[:], I["diff_lambda"][l].partition_broadcast(128), writes=[blam])
            P.tt("dve", lv[:, 0:64], lv[:, 0:64], lv[:, 64:128], ALU.mult, [blam], [blam])
            P.tt("dve", lv[:, 128:192], lv[:, 128:192], lv[:, 192:256], ALU.mult, [blam], [blam])
            P.reduce(lsc[:, 0:1], lv[:, 0:64], ALU.add, [blam], [blam]); P.reduce(lsc[:, 1:2], lv[:, 128:192], ALU.add, [blam], [blam])
            P.act(lsc[:, 2:4], lsc[:, 0:2], AF.Exp, [blam], [blam])
            P.tt("dve", nlam[:], lsc[:, 3:4], lsc[:, 2:3], ALU.subtract, [blam], [blam])
            P.ts("dve", nlam[:], nlam[:], -lam_init, None, ALU.add, reads=[blam], writes=[blam])
            gcs = P.sb("Cg", [128, 4], F32, ph)
            vec_fm(gcs[:], I["diff_norm_g"][l], writes=[blam])
            P.ts("dve", gcs[:], gcs[:], 1.0 - lam_init, None, ALU.mult, reads=[blam], writes=[blam])
            V = P.sb("CV", [128, 18, 4, 129], BF16, ph); bV = Buf()
            P.memset("pool", V[:, :, :, 128:129], 1.0, [bV])
            vst = [P.sb("Cvst%d" % i, [128, 512], F32, ph) for i in range(2)]; bvst = [Buf(), Buf()]
            for t in range(18):
                P.dma(vst[t % 2][:], PT_d[t * 128:(t + 1) * 128, TMO["Cv"]:TMO["Cv"] + 512], reads=[bPT], writes=[bvst[t % 2]], q="sp" if t % 2 else "pool")
                P.copy("act" if t % 2 else "dve", V[:, t, :, 0:128], vst[t % 2][:].rearrange("p (h d) -> p h d", h=4), [bvst[t % 2]], [bV])
            q32_ = [P.sb("Cq32%d" % i, [128, NT], F32, ph) for i in range(2)]; k32_ = [P.sb("Ck32%d" % i, [128, NT], F32, ph) for i in range(2)]
            bq32_ = [Buf(), Buf()]; bk32_ = [Buf(), Buf()]
            qr_ = [P.sb("Cqr%d" % i, [128, NT], BF16, ph) for i in range(2)]; kr_ = [P.sb("Ckr%d" % i, [128, NT], BF16, ph) for i in range(2)]
            bqr_ = [Buf(), Buf()]; bkr_ = [Buf(), Buf()]
            t1 = P.sb("Ct1", [128, 512], F32, ph); t2 = P.sb("Ct2", [128, 512], F32, ph); bt1 = Buf(); bt2 = Buf()
            pTs = [[P.sb("CpT%d%d" % (c, i), [128, 512], BF16, ph) for i in range(2)] for c in range(2)]; bpT = [[Buf(), Buf()], [Buf(), Buf()]]
            oo_ = [P.sb("Coo%d" % i, [128, 4, 128], F32, ph) for i in range(2)]; boo_ = [Buf(), Buf()]
            tq = P.sb("Ctq", [128, 4, 128], F32, ph); btq = Buf()
            rc = P.sb("Crc", [128, 2, 4], F32, ph); ssq = P.sb("Cssq", [128, 4], F32, ph); brc = Buf()
            yst_ = [P.sb("Cyst%d" % i, [128, NT], BF16, ph) for i in range(2)]; byst_ = [Buf(), Buf()]
            posf = P.sb("Cposf", [128, 8, 129], F32, ph); bposf = Buf()
            ps_s = [[P.ps("Cps%d%d" % (c, i), [128, 512], F32, ph) for i in range(2)] for c in range(2)]; bps_s = [[Buf(), Buf()], [Buf(), Buf()]]
            pob = [P.ps("Cpob%d" % i, [128, 512], F32, ph) for i in range(3)]; bpob = [Buf() for _ in range(3)]
            ps_t = P.ps("Cpst", [128, 512], F32, ph); bps_t = Buf()
            ps_r = ps_s[0][0]; bps_r = bps_s[0][0]
            pend = []
            blkno = [0]
            ssq2_ = [P.sb("Cssq2%d" % i, [128, 4], F32, ph) for i in range(2)]; bssq2_ = [Buf(), Buf()]
            si = 0; pi_ = 0
            def prepC(h):
                q32 = q32_[h % 2]; k32 = k32_[h % 2]; bq32 = bq32_[h % 2]; bk32 = bk32_[h % 2]
                P.dma(q32[:], PF_d[FMI["Cq"] + h], reads=[bPF[FMI["Cq"] + h]], writes=[bq32])
                P.dma(k32[:], PF_d[FMI["Ck"] + h], reads=[bPF[FMI["Ck"] + h]], writes=[bk32], q="pool")
                for (src, bsrc, dst, bdst) in ((q32, bq32, qr_[h % 2], bqr_[h % 2]), (k32, bk32, kr_[h % 2], bkr_[h % 2])):
                    P.copy("pool", dst[:, 0:NCTX], src[:, 0:NCTX], [bsrc], [bdst])
                    for j in range(4):
                        c0 = NCTX + j * 512
                        P.mm(ps_r[:], pm[:], src[:, c0:c0 + 512], True, True, [bcc, bsrc], [bps_r])
                        P.tt("dve", t1[:], ps_r[:], sinT[:, j * 512:(j + 1) * 512], ALU.mult, [bps_r, bcc], [bt1])
                        P.tt("pool", t2[:], src[:, c0:c0 + 512], cosT[:, j * 512:(j + 1) * 512], ALU.mult, [bsrc, bcc], [bt2])
                        P.tt("dve", dst[:, c0:c0 + 512], t1[:], t2[:], ALU.add, [bt1, bt2], [bdst])

            prepC(0)
            for h in range(4):
                if h + 1 < 4:
                    prepC(h + 1)
                qr = qr_[h % 2]; kr = kr_[h % 2]; bqr = bqr_[h % 2]; bkr = bkr_[h % 2]
                yst = yst_[h % 2]; byst = byst_[h % 2]
                for bix, (b0, bs) in enumerate(qblocks):
                    kts = list(range(2)) if b0 < NCTX else list(range(18))
                    nk = len(kts); nsub = bs // 128
                    oo = oo_[blkno[0] % 2]; boo = boo_[blkno[0] % 2]; blkno[0] += 1

                    def S_(ii):
                        kt = kts[ii]
                        for c in range(2):
                            P.mm(ps_s[c][ii % 2][:, :bs], kr[c * 64:(c + 1) * 64, kt * 128:(kt + 1) * 128], qr[c * 64:(c + 1) * 64, b0:b0 + bs], True, True, [bkr, bqr], [bps_s[c][ii % 2]])

                    S_(0)
                    started = set()
                    for ii, kt in enumerate(kts):
                        if ii + 1 < nk:
                            S_(ii + 1)
                        for c in range(2):
                            P.act(pTs[c][ii % 2][:, :bs], ps_s[c][ii % 2][:, :bs], AF.Exp, [bps_s[c][ii % 2]], [bpT[c][ii % 2]], scale=0.125)
                        for c in range(2):
                            for j in range(nsub):
                                f = c * 4 + j; bk = f // 3; off = (f % 3) * 129
                                st_ = (ii == 0 and bk not in started)
                                started.add(bk)
                                P.mm(pob[bk][:, off:off + 129], pTs[c][ii % 2][:, j * 128:(j + 1) * 128], V[:, kt, h, :], st_, ii == nk - 1,
                                     [bpT[c][ii % 2], bV], [bpob[bk]], skip_group_check=True)
                        if ii == min(9, nk - 1) and pend:
                            pend.pop(0)()
                    for c in range(2):
                        for j in range(nsub):
                            f = c * 4 + j; bk = f // 3; off = (f % 3) * 129
                            P.copy("dve", posf[:, f, :], pob[bk][:, off:off + 129], [bpob[bk]], [bposf])
                    for c in range(2):
                        P.recip(rc[:, c, :nsub], posf[:, 4 * c:4 * c + nsub, 128], [bposf], [brc])
                    P.ts("dve", rc[:, 1, :nsub], rc[:, 1, :nsub], nlam[:], None, ALU.mult, reads=[brc, blam], writes=[brc])
                    P.tt("dve", oo[:, :nsub], posf[:, 0:nsub, 0:128], rc[:, 0, :nsub].unsqueeze(2).to_broadcast([128, nsub, 128]), ALU.mult, [bposf, brc], [boo])
                    P.tt("dve", tq[:, :nsub], posf[:, 4:4 + nsub, 0:128], rc[:, 1, :nsub].unsqueeze(2).to_broadcast([128, nsub, 128]), ALU.mult, [bposf, brc], [btq])
                    P.tt("dve", oo[:, :nsub], oo[:, :nsub], tq[:, :nsub], ALU.add, [boo, btq], [boo])
                    P.tt("dve", tq[:, :nsub], oo[:, :nsub], oo[:, :nsub], ALU.mult, [boo], [btq])
                    P.reduce(ssq[:, :nsub], tq[:, :nsub], ALU.add, [btq], [brc])
                    ssq2 = ssq2_[blkno[0] % 2]; bssq2 = bssq2_[blkno[0] % 2]
                    P.copy("dve", ssq2[:, :nsub], ssq[:, :nsub], [brc], [bssq2])

                    def part2(oo=oo, boo=boo, nsub=nsub, b0=b0, bs=bs, h=h, yst=yst, byst=byst, lastblk=(bix == len(qblocks) - 1), ssq2=ssq2, bssq2=bssq2):
                        P.act(ssq2[:, :nsub], ssq2[:, :nsub], AF.Sqrt, [bssq2, bconst], [bssq2], bias=epsT[:], scale=1.0 / 128)
                        P.recip(ssq2[:, :nsub], ssq2[:, :nsub], [bssq2], [bssq2])
                        P.tt("dve", oo[:, :nsub], oo[:, :nsub], ssq2[:, :nsub].unsqueeze(2).to_broadcast([128, nsub, 128]), ALU.mult, [boo, bssq2], [boo])
                        for j in range(nsub):
                            P.tr(ps_t[:, j * 128:(j + 1) * 128], oo[:, j, :], ident[:], [boo, bconst], [bps_t])
                        P.ts("dve", yst[:, b0:b0 + bs], ps_t[:, :bs], gcs[:, h:h + 1], None, ALU.mult, reads=[bps_t, blam], writes=[byst])
                        if lastblk:
                            c_lo = qblocks[0][0]
                            P.dma(Y_d[2, :, h, c_lo:NT], yst[:, c_lo:NT], reads=[byst], writes=[bY[2]])

                    pend.append(part2)
            while pend:
                pend.pop(0)()
            P.barrier()
        if stop_after == "C":
            P.emit(); return nc

        with ES() as ph:
            rst = P.sb("Arst", [128, NT], F32, ph); bca = Buf()
            P.dma(rst[:], C["rst64"], writes=[bca])
            trm = [P.sb("Atri%d" % i, [64, 64], F32, ph) for i in range(2)]
            P.dma(trm[0][:], C["tri_f"][0:64, 0:64], writes=[bca]); P.dma(trm[1][:], C["tri_bsw"], writes=[bca])
            trmi = [P.sb("Atrii%d" % i, [64, 64], I32, ph) for i in range(2)]
            P.copy("dve", trmi[0][:], trm[0][:], [bca], [bca]); P.copy("dve", trmi[1][:], trm[1][:], [bca], [bca])
            lbr = P.sb("Albr", [128, 2, 2, 4], F32, ph); lb1 = P.sb("Alb1", [128, 2, 4], F32, ph); oml = P.sb("Aoml", [128, 2, 4], F32, ph)
            P.dma(lbr[:], I["hgrn_lb_raw"].rearrange("l d (h p) -> p l d h", p=128), writes=[bca], allow_slow_non_contiguous=True)
            P.tt("dve", lb1[:], lbr[:, 1], lbr[:, 0], ALU.subtract, [bca], [bca])
            P.act(lb1[:], lb1[:], AF.Sigmoid, [bca], [bca])
            P.ts("dve", oml[:], lb1[:], -1.0, 1.0, ALU.mult, ALU.add, reads=[bca], writes=[bca])
            ga = P.sb("Aga", [128, 4], F32, ph)
            vec_fm(ga[:], I["hgrn_norm_g"][l], writes=[bca])
            q32 = P.sb("Aq32", [128, NT], F32, ph); bq32 = Buf()
            T = [P.sb("AT%d" % i, [128, NT], F32, ph) for i in range(5)]; bT = [Buf() for _ in range(5)]
            qt = [P.sb("Aqt%d" % i, [128, NT], BF16, ph) for i in range(2)]; kt = [P.sb("Akt%d" % i, [128, NT], BF16, ph) for i in range(2)]
            qh = [P.sb("Aqh%d" % i, [128, NT], BF16, ph) for i in range(2)]; kh = [P.sb("Akh%d" % i, [128, NT], BF16, ph) for i in range(2)]
            bqk = [Buf(), Buf()]
            khT = [P.sb("AkhT%d" % i, [64, 36, 128], BF16, ph) for i in range(2)]; bkhT = [Buf(), Buf()]
            V64s = P.sb("AV64s", [64, 36, 128], F32, ph); V64b = P.sb("AV64b", [64, 36, 128], BF16, ph); bV64s = Buf(); bV64 = Buf()
            oacc = P.sb("Aoacc", [128, NT], F32, ph); boacc = Buf()
            V64sw = P.sb("AV64sw", [64, 36, 128], BF16, ph); bV64sw = Buf()
            bend = P.sb("Abend", [128, 36], F32, ph); bbend = Buf()
            dec = [P.sb("Adec%d" % i, [128, 36], F32, ph) for i in range(2)]; bdec = [Buf(), Buf()]
            S = [P.sb("AS%d" % i, [128, 128], F32, ph) for i in range(2)]; Sb = [P.sb("ASb%d" % i, [128, 128], BF16, ph) for i in range(2)]
            bS = [Buf(), Buf()]; bSb = [Buf(), Buf()]
            pT = [[P.sb("ApT%d%d" % (i, j), [64, 64], BF16, ph) for j in range(2)] for i in range(2)]; bpT = [[Buf(), Buf()], [Buf(), Buf()]]
            for i_ in range(2):
                for j_ in range(2):
                    P.memset("dve", pT[i_][j_][:], 0.0, [bpT[i_][j_]])
            rr_ = [P.sb("Arr%d" % i, [128, 512], F32, ph) for i in range(2)]; tq_ = [P.sb("Atq%d" % i, [128, 512], F32, ph) for i in range(2)]
            brr_ = [Buf(), Buf()]; btq_ = [Buf(), Buf()]; sgl_ = [P.sb("Asgl%d" % i, [128, 512], F32, ph) for i in range(2)]; bsgl_ = [Buf(), Buf()]
            yst = P.sb("Ayst", [128, NT], BF16, ph); byst = Buf()
            psc = [P.ps("Apsc%d" % i, [128, 512], F32, ph) for i in range(2)]; bpsc = [Buf(), Buf()]
            pso = [P.ps("Apso%d" % i, [128, 512], F32, ph) for i in range(2)]; bpso = [Buf(), Buf()]
            pst = [P.ps("Apst%d" % i, [128, 512], F32, ph) for i in range(2)]; bpst = [Buf(), Buf()]
            ptr = P.ps("Aptr", [128, 1024], BF16, ph); bptr = Buf()
            pn = P.ps("Apn", [128, 512], F32, ph); bpn = Buf()
            for i_ in range(2):
                P.memset("dve", psc[i_][:], 0.0, [bpsc[i_]])
            G = [(0, 4), (4, 12), (12, 20), (20, 28), (28, 36)]
            seqs = [[n for (g0, g1) in G for n in range(g0, g1)],
                    [n for (g0, g1) in [G[0], G[4], G[3], G[2], G[1]] for n in range(g1 - 1, g0 - 1, -1)]]
            grp = {n: g for g in G for n in range(g[0], g[1])}

            def v3(t_):
                return t_[:].rearrange("p (n j) -> p n j", j=64)

            for h in range(4):
                P.dma(q32[:], PF_d[FMI["Aq"] + h], reads=[bPF[FMI["Aq"] + h]], writes=[bq32])
                P.dma(V64s[:], PT_d[:, TMO["Ai"] + h * 128:TMO["Ai"] + (h + 1) * 128].rearrange("(c p) d -> p c d", p=64), reads=[bPT], writes=[bV64s], q="pool")
                P.copy("act", V64b[:, 0:18], V64s[:, 0:18], [bV64s], [bV64]); P.copy("dve", V64b[:, 18:36], V64s[:, 18:36], [bV64s], [bV64])
                vsrc = PT_d[:, TMO["Ai"] + h * 128:TMO["Ai"] + (h + 1) * 128].rearrange("(c h j) d -> h j c d", h=2, j=32)
                P.dma(V64s[0:32], vsrc[1], reads=[bPT], writes=[bV64s]); P.dma(V64s[32:64], vsrc[0], reads=[bPT], writes=[bV64s], q="pool")
                P.copy("act", V64sw[:, 0:18], V64s[:, 0:18], [bV64s], [bV64sw]); P.copy("dve", V64sw[:, 18:36], V64s[:, 18:36], [bV64s], [bV64sw])
                for d in range(2):
                    sgn = 1.0 if d == 0 else -1.0
                    zi = FMI["Aff" if d == 0 else "Afb"] + h
                    HV = [(0, 1152), (1152, 2304)]
                    bTh = [[Buf(), Buf()] for _ in range(5)]
                    bbh = [Buf(), Buf()]
                    for i_ in range(5):
                        for hf in range(2):
                            bTh[i_][hf].w = bT[i_].w; bTh[i_][hf].r = dict(bT[i_].r)
                    for hf in range(2):
                        bbh[hf].w = bbend.w; bbh[hf].r = dict(bbend.r)

                    def c3(t_, a, b):
                        return t_[:, a:b].rearrange("p (n j) -> p n j", j=64)

                    def c4(t_, a, b):
                        return t_[:, a:b].rearrange("p (n h j) -> p n h j", h=2, j=32)

                    def each(fn):
                        for hf, (a, b) in enumerate(HV):
                            fn(hf, a, b, slice(hf * 18, (hf + 1) * 18))

                    each(lambda hf, a, b, ns: P.dma(T[0][:, a:b], PF_d[zi][:, a:b], reads=[bPF[zi]], writes=[bTh[0][hf]], q="sp" if hf == 0 else "pool"))
                    each(lambda hf, a, b, ns: P.act(T[0][:, a:b], T[0][:, a:b], AF.Sigmoid, [bTh[0][hf]], [bTh[0][hf]]))
                    if l == 1:
                        each(lambda hf, a, b, ns: P.ts("dve", T[0][:, a:b], T[0][:, a:b], oml[:, d, h:h + 1], lb1[:, d, h:h + 1], ALU.mult, ALU.add, reads=[bTh[0][hf], bca], writes=[bTh[0][hf]]))
                    each(lambda hf, a, b, ns: P.act(T[1][:, a:b], T[0][:, a:b], AF.Ln, [bTh[0][hf]], [bTh[1][hf]]))
                    each(lambda hf, a, b, ns: P.ts("pool", T[2][:, a:b], T[0][:, a:b], -1.0, 1.0, ALU.mult, ALU.add, reads=[bTh[0][hf]], writes=[bTh[2][hf]]))
                    each(lambda hf, a, b, ns: P.op("dve", (lambda a=a, b=b: (lambda e: e.tensor_tensor_scan(T[3][:, a:b], rst[:, a:b], T[1][:, a:b], 0.0, ALU.mult, ALU.add)))(), [bca, bTh[1][hf]], [bTh[3][hf]]))
                    each(lambda hf, a, b, ns: P.copy("dve", bend[:, ns], c3(T[3], a, b)[:, :, 63], [bTh[3][hf]], [bbh[hf]]))
                    each(lambda hf, a, b, ns: P.act(dec[d][:, ns], bend[:, ns], AF.Exp, [bbh[hf]], [bdec[d]]))
                    if d == 1:
                        each(lambda hf, a, b, ns: P.tt("dve", T[3][:, a:b], T[3][:, a:b], T[1][:, a:b], ALU.subtract, [bTh[3][hf], bTh[1][hf]], [bTh[3][hf]]))
                    each(lambda hf, a, b, ns: P.tt("dve", c3(T[4], a, b), c3(T[3], a, b), c3(T[3], a, b)[:, :, 31:32].to_broadcast([128, 18, 64]), ALU.subtract, [bTh[3][hf]], [bTh[4][hf]]))
                    each(lambda hf, a, b, ns: P.act(T[1][:, a:b], T[4][:, a:b], AF.Exp, [bTh[4][hf]], [bTh[1][hf]], scale=sgn))
                    each(lambda hf, a, b, ns: P.tt("dve", qt[d][:, a:b], q32[:, a:b], T[1][:, a:b], ALU.mult, [bq32, bTh[1][hf]], [bqk[d]]))
                    each(lambda hf, a, b, ns: P.act(T[1][:, a:b], T[4][:, a:b], AF.Exp, [bTh[4][hf]], [bTh[1][hf]], scale=-sgn))

                    def kmul(dst):
                        def f(hf, a, b, ns):
                            if d == 0:
                                P.tt("dve", dst[:, a:b], T[2][:, a:b], T[1][:, a:b], ALU.mult, [bTh[2][hf], bTh[1][hf]], [bqk[d]])
                            else:
                                for hh in range(2):
                                    P.tt("dve", c4(dst, a, b)[:, :, 1 - hh, :], c4(T[2], a, b)[:, :, hh, :], c4(T[1], a, b)[:, :, hh, :], ALU.mult, [bTh[2][hf], bTh[1][hf]], [bqk[d]])
                        return f

                    def qmul(dst):
                        return lambda hf, a, b, ns: P.tt("dve", dst[:, a:b], q32[:, a:b], T[1][:, a:b], ALU.mult, [bq32, bTh[1][hf]], [bqk[d]])

                    each(kmul(kt[d]))
                    each(lambda hf, a, b, ns: P.act(T[1][:, a:b], T[3][:, a:b], AF.Exp, [bTh[3][hf]], [bTh[1][hf]]))
                    each(qmul(qh[d]) if d == 0 else kmul(kh[d]))
                    each(lambda hf, a, b, ns: P.tt("dve", c3(T[4], a, b), c3(T[3], a, b), bend[:, ns].unsqueeze(2).to_broadcast([128, 18, 64]), ALU.subtract, [bTh[3][hf], bbh[hf]], [bTh[4][hf]]))
                    each(lambda hf, a, b, ns: P.act(T[1][:, a:b], T[4][:, a:b], AF.Exp, [bTh[4][hf]], [bTh[1][hf]], scale=-1.0))
                    each(kmul(kh[d]) if d == 0 else qmul(qh[d]))
                    for i_ in range(5):
                        for hf in range(2):
                            bb = bTh[i_][hf]
                            if bb.w is not None and (bT[i_].w is None or True):
                                pass
                        mw = {}
                        for hf in range(2):
                            bb = bTh[i_][hf]
                            if bb.w is not None:
                                mw[bb.w[0]] = max(mw.get(bb.w[0], 0), bb.w[1])
                            for k_, v_ in bb.r.items():
                                mw[k_] = max(mw.get(k_, 0), v_)
                        bT[i_].w = None; bT[i_].r = mw
                    mw = {}
                    for hf in range(2):
                        if bbh[hf].w is not None:
                            mw[bbh[hf].w[0]] = max(mw.get(bbh[hf].w[0], 0), bbh[hf].w[1])
                        for k_, v_ in bbh[hf].r.items():
                            mw[k_] = max(mw.get(k_, 0), v_)
                    bbend.w = None; bbend.r = mw
                    for g8 in range(0, 36, 8):
                        ng = min(8, 36 - g8)
                        for j in range(ng):
                            n = g8 + j
                            P.tr(ptr[0:64, j * 128:(j + 1) * 128], kh[d][:, n * 64:(n + 1) * 64], identb[:], [bqk[d], bconst], [bptr])
                        P.copy("act", khT[d][:, g8:g8 + ng, :], ptr[0:64, 0:ng * 128].rearrange("p (n c) -> p n c", c=128), [bptr], [bkhT[d]])
                P.memset("pool", oacc[:], 0.0, [boacc])
                first = [True, True]
                for i in range(36):
                    for d in range(2):
                        n = seqs[d][i]
                        g0, g1 = grp[n]
                        cs_ = slice(n * 64, (n + 1) * 64)
                        sl = i % 8
                        c0 = sl * 64; n0 = n * 64
                        if d == 0:
                            P.mm(psc[d][0:32, c0:c0 + 32], kt[d][:, n0:n0 + 32], qt[d][:, n0:n0 + 32], True, True, [bqk[d]], [bpsc[d]])
                            P.mm(psc[d][0:64, c0 + 32:c0 + 64], kt[d][:, n0:n0 + 64], qt[d][:, n0 + 32:n0 + 64], True, True, [bqk[d]], [bpsc[d]])
                        else:
                            P.mm(psc[d][0:32, c0 + 32:c0 + 64], kt[d][:, n0:n0 + 32], qt[d][:, n0 + 32:n0 + 64], True, True, [bqk[d]], [bpsc[d]])
                            P.mm(psc[d][0:64, c0:c0 + 32], kt[d][:, n0:n0 + 64], qt[d][:, n0:n0 + 32], True, True, [bqk[d]], [bpsc[d]])
                        pt_ = pT[d][i % 2]; bpt_ = bpT[d][i % 2]
                        P.op("dve", (lambda o_, m_, d_: (lambda e: e.copy_predicated(o_, m_, d_)))(pt_[:], trmi[d][:], psc[d][0:64, sl * 64:(sl + 1) * 64]), [bpsc[d], bca, bpt_], [bpt_])
                        col = (n - g0) * 64
                        Vd = V64b if d == 0 else V64sw; bVd = bV64 if d == 0 else bV64sw
                        P.mm(pso[d][:, col:col + 64], Vd[:, n, :], pt_[:], True, first[d], [bVd, bpt_], [bpso[d]])
                        if not first[d]:
                            P.mm(pso[d][:, col:col + 64], Sb[d][:], qh[d][:, cs_], False, True, [bSb[d], bqk[d]], [bpso[d]])
                        ss = i % 4
                        P.mm(pst[d][:, ss * 128:(ss + 1) * 128], khT[d][:, n, :], Vd[:, n, :], True, True, [bkhT[d], bVd], [bpst[d]])
                        if first[d]:
                            P.copy("dve", S[d][:], pst[d][:, ss * 128:(ss + 1) * 128], [bpst[d]], [bS[d]])
                        else:
                            P.stt(S[d][:], S[d][:], dec[d][:, n:n + 1], pst[d][:, ss * 128:(ss + 1) * 128], ALU.mult, ALU.add, [bS[d], bdec[d], bpst[d]], [bS[d]])
                        P.copy("act", Sb[d][:], S[d][:], [bS[d]], [bSb[d]])
                        first[d] = False
                        lastn = (g1 - 1) if d == 0 else g0
                        if n == lastn:
                            w_ = (g1 - g0) * 64
                            P.tt("dve", oacc[:, g0 * 64:g1 * 64], pso[d][:, :w_], oacc[:, g0 * 64:g1 * 64], ALU.add, [bpso[d], boacc], [boacc])
                gi_ = FMI["Ag"] + h
                P.dma(T[0][:], PF_d[gi_], reads=[bPF[gi_]], writes=[bT[0]])
                for bi_, (b0, bs) in enumerate(qblocks):
                    rr = rr_[bi_ % 2]; brr = brr_[bi_ % 2]; tq = tq_[bi_ % 2]; btq = btq_[bi_ % 2]; sgl = sgl_[bi_ % 2]; bsgl = bsgl_[bi_ % 2]
                    P.act(tq[:, :bs], oacc[:, b0:b0 + bs], AF.Square, [boacc], [btq])
                    P.mm(pn[:, :bs], ones[:], tq[:, :bs], True, True, [bconst, btq], [bpn])
                    P.act(rr[:, :bs], pn[:, :bs], AF.Sqrt, [bpn, bconst], [brr], bias=epsT[:], scale=1.0 / 128)
                    P.recip(rr[:, :bs], rr[:, :bs], [brr], [brr])
                    P.tt("dve", tq[:, :bs], oacc[:, b0:b0 + bs], rr[:, :bs], ALU.mult, [boacc, brr], [btq])
                    P.act(sgl[:, :bs], T[0][:, b0:b0 + bs], AF.Silu, [bT[0]], [bsgl])
                    P.stt(yst[:, b0:b0 + bs], tq[:, :bs], ga[:, h:h + 1], sgl[:, :bs], ALU.mult, ALU.mult, [btq, bca, bsgl], [byst])
                c_lo = qblocks[0][0]
                P.dma(Y_d[0, :, h, c_lo:NT], yst[:, c_lo:NT], reads=[byst], writes=[bY[0]])
            P.barrier()
        if stop_after == "A":
            P.emit(); return nc

        with ES() as ph:
            bcb = Buf()
            trm = [P.sb("Btri%d" % i, [128, 128], F32, ph) for i in range(2)]
            P.dma(trm[0][:], C["tri_f"], writes=[bcb]); P.dma(trm[1][:], C["tri_b"], writes=[bcb])
            G_ = P.sb("BG", [128, 18, 16], F32, ph); gb = P.sb("Bgb", [128, 16], F32, ph); bG = Buf()
            P.dma(G_[:], PT_d[:, TMO["Bg"]:TMO["Bg"] + 16].rearrange("(n p) g -> p n g", p=128), reads=[bPT], writes=[bG], allow_slow_non_contiguous=True)
            P.dma(gb[:], I["mlstm_gate_b"][l].partition_broadcast(128), writes=[bcb])
            P.tt("dve", G_[:], G_[:], gb[:].unsqueeze(1).to_broadcast([128, 18, 16]), ALU.add, [bG, bcb], [bG])
            LF = P.sb("BLF", [128, 18, 8], F32, ph); LI = P.sb("BLI", [128, 18, 8], F32, ph); bLF = Buf()
            P.act(LF[:], G_[:, :, 8:16], AF.Sigmoid, [bG], [bLF])
            P.act(LF[:], LF[:], AF.Ln, [bLF], [bLF])
            P.copy("dve", LI[:], G_[:, :, 0:8], [bG], [bLF])
            GI = P.sb("BGI", [128, 18, 8], F32, ph); TT = P.sb("BTT", [128, 18, 8], F32, ph)
            A_ = P.sb("BA", [128, 18, 8], F32, ph); Cc = P.sb("BC", [128, 18, 8], F32, ph); Ee = P.sb("BE", [128, 18, 8], F32, ph); DT = P.sb("BDT", [128, 18, 8], F32, ph)
            bsc = Buf()
            psS = [P.ps("BpsS%d" % i, [128, 512], F32, ph) for i in range(2)]; bpsS = [Buf(), Buf()]
            psH = [P.ps("BpsH%d" % i, [128, 512], F32, ph) for i in range(2)]; bpsH = [Buf(), Buf()]
            psC = [P.ps("BpsC%d" % i, [128, 512], F32, ph) for i in range(2)]; bpsC = [Buf(), Buf()]
            pkt = P.ps("Bpkt", [128, 1024], BF16, ph); bpkt = Buf()
            pfin = P.ps("Bpfin", [128, 512], F32, ph); bpfin = Buf()
            LFf = LF[:].rearrange("p n j -> p (n j)")
            P.mm(psS[0][:, 0:144], trm[0][:], LFf, True, True, [bcb, bLF], [bpsS[0]])
            P.mm(psS[1][:, 0:144], ones[:], LFf, True, True, [bconst, bLF], [bpsS[1]])
            P.copy("dve", GI[:].rearrange("p n j -> p (n j)"), psS[0][:, 0:144], [bpsS[0]], [bsc])
            P.copy("dve", TT[:].rearrange("p n j -> p (n j)"), psS[1][:, 0:144], [bpsS[1]], [bsc])
            P.copy("dve", A_[:, :, 0:4], GI[:, :, 0:4], [bsc], [bsc])
            P.tt("dve", A_[:, :, 4:8], TT[:, :, 4:8], GI[:, :, 4:8], ALU.subtract, [bsc], [bsc])
            P.tt("dve", A_[:, :, 4:8], A_[:, :, 4:8], LF[:, :, 4:8], ALU.add, [bsc, bLF], [bsc])
            P.tt("dve", Cc[:], LI[:], A_[:], ALU.subtract, [bsc, bLF], [bsc])
            P.act(Cc[:], Cc[:], AF.Exp, [bsc], [bsc])
            P.tt("dve", Ee[:, :, 0:4], TT[:, :, 0:4], GI[:, :, 0:4], ALU.subtract, [bsc], [bsc])
            P.tt("dve", Ee[:, :, 4:8], GI[:, :, 4:8], LF[:, :, 4:8], ALU.subtract, [bsc, bLF], [bsc])
            P.tt("dve", Ee[:], Ee[:], LI[:], ALU.add, [bsc, bLF], [bsc])
            P.act(Ee[:], Ee[:], AF.Exp, [bsc], [bsc])
            P.act(DT[:], TT[:], AF.Exp, [bsc], [bsc])
            P.act(A_[:], A_[:], AF.Exp, [bsc], [bsc])
            cw = P.sb("Bcw", [128, 3, 8], F32, ph); cb = P.sb("Bcb", [128, 8], F32, ph)
            P.dma(cw[:], I["mlstm_conv_w"][l].rearrange("j (c p) -> p j c", p=128), writes=[bcb], allow_slow_non_contiguous=True)
            vec_fm(cb[:], I["mlstm_conv_b"][l], writes=[bcb])
            gB = P.sb("BgB", [128, 512], F32, ph)
            P.dma(gB[:], I["mlstm_norm_g"][l].partition_broadcast(128), writes=[bcb])
            V1 = P.sb("BV1", [128, 18, 4, 129], BF16, ph); bV1 = Buf()
            P.memset("pool", V1[:, :, :, 128:129], 1.0, [bV1])
            vst = [P.sb("Bvst%d" % i, [128, 512], F32, ph) for i in range(2)]; bvst = [Buf(), Buf()]
            for t in range(18):
                P.dma(vst[t % 2][:], PT_d[t * 128:(t + 1) * 128, TMO["Bv"]:TMO["Bv"] + 512], reads=[bPT], writes=[bvst[t % 2]], q="sp" if t % 2 else "pool")
                P.copy("act" if t % 2 else "dve", V1[:, t, :, 0:128], vst[t % 2][:].rearrange("p (h d) -> p h d", h=4), [bvst[t % 2]], [bV1])
            x32 = [P.sb("Bx%d" % i, [128, NT], F32, ph) for i in range(2)]; bx32 = [Buf(), Buf()]
            cv = [P.sb("Bcv%d" % i, [128, NT], F32, ph) for i in range(2)]; bcv = [Buf(), Buf()]
            qT = P.sb("BqT", [128, NT], BF16, ph); kT = P.sb("BkT", [128, NT], BF16, ph); bqT = Buf(); bkT = Buf()
            ktm = [P.sb("Bktm%d" % i, [128, 18, 128], BF16, ph) for i in range(2)]; bktm = [Buf(), Buf()]
            hacc = P.sb("Bhacc", [128, 18, 512], F32, ph); bhacc = Buf()
            P.memset("pool", hacc[:], 0.0, [bhacc])
            CN = [P.sb("BCN%d" % i, [128, 129], F32, ph) for i in range(2)]; CNb = [P.sb("BCNb%d" % i, [128, 129], BF16, ph) for i in range(2)]
            bCN = [Buf(), Buf()]; bCNb = [Buf(), Buf()]
            pT = [[P.sb("BpT%d%d" % (i, j), [128, 128], BF16, ph) for j in range(2)] for i in range(2)]; bpT = [[Buf(), Buf()], [Buf(), Buf()]]
            ND = P.sb("BND", [128, 18, 2, 129], F32, ph); bND = Buf()
            rd = P.sb("Brd", [128, 18, 2], F32, ph); rd2 = P.sb("Brd2", [128, 18, 2], F32, ph)
            CNb2 = [[P.sb("BCNb%d%d" % (i, j), [128, 129], BF16, ph) for j in range(2)] for i in range(2)]; bCNb2 = [[Buf(), Buf()], [Buf(), Buf()]]
            seqs = [list(range(18)), [1, 0] + list(range(17, 1, -1))]
            for h in range(4):
                for w_, (nm, cc) in enumerate((("Bq", h), ("Bk", 4 + h))):
                    xi = FMI[nm] + h
                    x_ = x32[w_]; o_ = cv[w_]
                    P.dma(x_[:], PF_d[xi], reads=[bPF[xi]], writes=[bx32[w_]], q="sp" if w_ == 0 else "pool")
                    P.ts("dve", o_[:], x_[:], cw[:, 1, cc:cc + 1], cb[:, cc:cc + 1], ALU.mult, ALU.add, reads=[bx32[w_], bcb], writes=[bcv[w_]])
                    for (a, b) in ((0, NCTX), (NCTX, NT)):
                        P.stt(o_[:, a + 1:b], x_[:, a:b - 1], cw[:, 0, cc:cc + 1], o_[:, a + 1:b], ALU.mult, ALU.add, [bx32[w_], bcb, bcv[w_]], [bcv[w_]])
                        P.stt(o_[:, a:b - 1], x_[:, a + 1:b], cw[:, 2, cc:cc + 1], o_[:, a:b - 1], ALU.mult, ALU.add, [bx32[w_], bcb, bcv[w_]], [bcv[w_]])
                    if w_ == 0:
                        P.act(o_[:], o_[:], AF.Silu, [bcv[w_]], [bcv[w_]])
                        P.ts("dve", qT[:], o_[:], 128.0 ** -0.5, None, ALU.mult, reads=[bcv[w_]], writes=[bqT])
                    else:
                        P.act(kT[:], o_[:], AF.Silu, [bcv[w_]], [bkT])
                for n in range(18):
                    sl = n % 8
                    P.tr(pkt[:, sl * 128:(sl + 1) * 128], kT[:, n * 128:(n + 1) * 128], identb[:], [bkT, bconst], [bpkt])
                    P.act(ktm[0][:, n, :], pkt[:, sl * 128:(sl + 1) * 128], AF.Copy, [bpkt, bsc], [bktm[0]], scale=Ee[:, n, h:h + 1])
                    P.ts("dve", ktm[1][:, n, :], pkt[:, sl * 128:(sl + 1) * 128], Ee[:, n, 4 + h:5 + h], None, ALU.mult, reads=[bpkt, bsc], writes=[bktm[1]])
                first = [True, True]
                for i in range(18):
                    for d in range(2):
                        n = seqs[d][i]; j = d * 4 + h
                        ts_ = slice(n * 128, (n + 1) * 128)
                        sl = i % 4
                        pss = psS[d][:, sl * 128:(sl + 1) * 128]
                        P.mm(pss, kT[:, ts_], qT[:, ts_], True, True, [bkT, bqT], [bpsS[d]])
                        pt_ = pT[d][i % 2]; bpt_ = bpT[d][i % 2]
                        P.stt(pt_[:], pss, Cc[:, n, j:j + 1], trm[d][:], ALU.mult, ALU.mult, [bpsS[d], bsc, bcb], [bpt_])
                        P.mm(psH[d][:, 0:129], pt_[:], V1[:, n, h, :], True, first[d], [bpt_, bV1], [bpsH[d]])
                        if not first[d]:
                            P.mm(psH[d][:, 0:129], qT[:, ts_], CNb2[d][(i + 1) % 2][:], False, True, [bqT, bCNb2[d][(i + 1) % 2]], [bpsH[d]])
                        P.copy("act", ND[:, n, d, :], psH[d][:, 0:129], [bpsH[d]], [bND])
                        P.mm(psC[d][:, 0:129], ktm[d][:, n, :], V1[:, n, h, :], True, True, [bktm[d], bV1], [bpsC[d]])
                        if first[d]:
                            P.copy("dve", CN[d][:], psC[d][:, 0:129], [bpsC[d]], [bCN[d]])
                        else:
                            P.stt(CN[d][:], CN[d][:], DT[:, n, j:j + 1], psC[d][:, 0:129], ALU.mult, ALU.add, [bCN[d], bsc, bpsC[d]], [bCN[d]])
                        P.copy("act", CNb2[d][i % 2][:], CN[d][:], [bCN[d]], [bCNb2[d][i % 2]])
                        first[d] = False
                av = A_[:, :, h:h + 5:4]
                P.tt("dve", ND[:], ND[:], av.unsqueeze(3).to_broadcast([128, 18, 2, 129]), ALU.mult, [bND, bsc], [bND])
                P.ts("dve", rd[:], ND[:, :, :, 128], -1.0, 1.0, ALU.mult, ALU.max, reads=[bND], writes=[bND])
                P.ts("dve", rd2[:], ND[:, :, :, 128], 1.0, None, ALU.max, reads=[bND], writes=[bND])
                P.tt("dve", rd[:], rd[:], rd2[:], ALU.max, [bND], [bND])
                P.recip(rd[:], rd[:], [bND], [bND])
                P.tt("dve", ND[:, :, :, 0:128], ND[:, :, :, 0:128], rd[:].unsqueeze(3).to_broadcast([128, 18, 2, 128]), ALU.mult, [bND], [bND])
                P.tt("dve", hacc[:, :, h * 128:(h + 1) * 128], ND[:, :, 0, 0:128], ND[:, :, 1, 0:128], ALU.add, [bND], [bhacc])
            sq = P.sb("Bsq", [128, 18, 512], F32, ph); bsq = Buf()
            ss = P.sb("Bss", [128, 72], F32, ph)
            P.tt("dve", sq[:], hacc[:], hacc[:], ALU.mult, [bhacc], [bsq])
            P.reduce(ss[:], sq[:].rearrange("p n (h d) -> p (n h) d", h=4), ALU.add, [bsq], [bsq])
            P.act(ss[:], ss[:], AF.Sqrt, [bsq, bconst], [bsq], bias=epsT[:], scale=1.0 / 128)
            P.recip(ss[:], ss[:], [bsq], [bsq])
            hv = hacc[:].rearrange("p n (h d) -> p (n h) d", h=4)
            P.tt("dve", hv, hv, ss[:].unsqueeze(2).to_broadcast([128, 72, 128]), ALU.mult, [bhacc, bsq], [bhacc])
            P.tt("dve", hacc[:], hacc[:], gB[:].unsqueeze(1).to_broadcast([128, 18, 512]), ALU.mult, [bhacc, bcb], [bhacc])
            yst = P.sb("Byst", [128, NT], BF16, ph); byst = Buf()
            for h in range(4):
                oi_ = FMI["Bo"] + h
                P.dma(x32[0][:], PF_d[oi_], reads=[bPF[oi_]], writes=[bx32[0]])
                P.act(x32[0][:], x32[0][:], AF.Sigmoid, [bx32[0]], [bx32[0]])
                for g4 in range(0, 18, 4):
                    ng = min(4, 18 - g4)
                    for jj in range(ng):
                        P.tr(pfin[:, jj * 128:(jj + 1) * 128], hacc[:, g4 + jj, h * 128:(h + 1) * 128], ident[:], [bhacc, bconst], [bpfin])
                    cs_ = slice(g4 * 128, (g4 + ng) * 128)
                    P.tt("dve", yst[:, cs_], pfin[:, 0:ng * 128], x32[0][:, cs_], ALU.mult, [bpfin, bx32[0]], [byst])
                c_lo = qblocks[0][0]
                P.dma(Y_d[1, :, h, c_lo:NT], yst[:, c_lo:NT], reads=[byst], writes=[bY[1]])
            P.barrier()
        if stop_after == "B":
            P.emit(); return nc

        with ES() as ph:
            wg = P.sb("Gwg", [128, 8, 3072], BF16, ph); wbr = P.sb("Gwbr", [128, 12, 1024], BF16, ph); wo = P.sb("Gwo", [128, 8, 1024], BF16, ph)
            bwts = Buf()
            bgt = P.sb("Gbg", [128, 24], F32, ph)
            vec_fm(bgt[:], I["b_gate"][l], writes=[bwts])
            stg = [P.sb("Gstg%d" % i, [128, 4096], F32, ph) for i in range(2)]; bstg = [Buf(), Buf()]
            bwg = [Buf() for _ in range(6)]; bwbr = [Buf() for _ in range(3)]; bwo = [Buf() for _ in range(2)]
            jobs = []

            def jg(g6):
                jobs.append((I["w_gate"][l, :, g6 * 512:(g6 + 1) * 512].rearrange("(k p) c -> p k c", p=128), 8, 512, wg[:, :, g6 * 512:(g6 + 1) * 512], bwg[g6]))

            def jb(m):
                jobs.append((I["w_branch"][l, m].rearrange("(k p) c -> p k c", p=128), 4, 1024, wbr[:, m * 4:(m + 1) * 4, :], bwbr[m]))

            jg(0); jb(0); jg(2); jb(1); jg(4); jb(2); jg(1); jg(3); jg(5)
            for g2_ in range(2):
                jobs.append((I["w_out"][l, :, g2_ * 512:(g2_ + 1) * 512].rearrange("(k p) c -> p k c", p=128), 8, 512, wo[:, :, g2_ * 512:(g2_ + 1) * 512], bwo[g2_]))
            for ji, (src, a_, b_, dst, bdst) in enumerate(jobs):
                i2 = ji % 2
                sv = stg[i2][:].rearrange("p (a b) -> p a b", a=a_)
                P.dma(sv, src, writes=[bstg[i2]], q="sp" if i2 == 0 else "pool")
                P.copy(("dve", "act")[ji % 2], dst, sv, [bstg[i2]], [bdst])
            hb = P.sb("Ghb", [128, 8, 512], BF16, ph); bhb = Buf()
            Yb = P.sb("GYb", [128, 3, 4, 512], BF16, ph); bYb = Buf()
            xb = P.sb("Gxb", [128, 8, 512], F32, ph); bxb = Buf()
            yb_ = P.sb("Gy", [128, 8, 512], BF16, ph); byb = Buf()
            sg = [P.sb("Gsg%d" % i, [128, 512], F32, ph) for i in range(2)]; bsg = [Buf(), Buf()]
            yacc = P.sb("Gyacc", [128, 512], F32, ph); byacc = Buf()
            tmp = P.sb("Gtmp", [128, 512], F32, ph); btmp = Buf()
            psg = [P.ps("Gpsg%d" % i, [128, 512], F32, ph) for i in range(3)]; bpsg = [Buf() for _ in range(3)]
            psb = [P.ps("Gpsb%d" % i, [128, 512], F32, ph) for i in range(3)]; bpsb = [Buf() for _ in range(3)]
            pso = [P.ps("Gpso%d" % i, [128, 512], F32, ph) for i in range(2)]; bpso = [Buf(), Buf()]
            gi = 0
            for (b0, bs) in qblocks:
                who = 1 if b0 < NCTX else 0
                P.dma(hb[:, :, :bs], HT_d[:, :, b0:b0 + bs], reads=[bHT], writes=[bhb])
                for m in range(3):
                    P.dma(Yb[:, m, :, :bs], Y_d[m, :, :, b0:b0 + bs], reads=[bY[m]], writes=[bYb], q="pool")
                P.dma(xb[:, :, :bs], XT_d[:, :, b0:b0 + bs], reads=[bXT], writes=[bxb])
                for oc in range(8):
                    for m in range(3):
                        pi = gi % 3; gi += 1
                        for kc in range(8):
                            P.mm(psg[pi][:, :bs], wg[:, kc, m * 1024 + oc * 128:m * 1024 + (oc + 1) * 128], hb[:, kc, :bs], kc == 0, kc == 7, [bwg[m * 2 + oc // 4], bhb], [bpsg[pi]])
                        for kc in range(4):
                            P.mm(psb[pi][:, :bs], wbr[:, m * 4 + kc, oc * 128:(oc + 1) * 128], Yb[:, m, kc, :bs], kc == 0, kc == 3, [bwbr[m], bYb], [bpsb[pi]])
                        s2 = gi % 2
                        P.act(sg[s2][:, :bs], psg[pi][:, :bs], AF.Sigmoid, [bpsg[pi], bwts], [bsg[s2]], bias=bgt[:, m * 8 + oc:m * 8 + oc + 1])
                        if m == 0:
                            P.tt("dve", yacc[:, :bs], sg[s2][:, :bs], psb[pi][:, :bs], ALU.mult, [bsg[s2], bpsb[pi]], [byacc])
                        else:
                            P.tt("dve", tmp[:, :bs], sg[s2][:, :bs], psb[pi][:, :bs], ALU.mult, [bsg[s2], bpsb[pi]], [btmp])
                            if m == 1:
                                P.tt("pool", yacc[:, :bs], yacc[:, :bs], tmp[:, :bs], ALU.add, [byacc, btmp], [byacc])
                            else:
                                P.tt("pool", yb_[:, oc, :bs], yacc[:, :bs], tmp[:, :bs], ALU.add, [byacc, btmp], [byb])
                for oc in range(8):
                    p2 = oc % 2
                    for kc in range(8):
                        P.mm(pso[p2][:, :bs], wo[:, kc, oc * 128:(oc + 1) * 128], yb_[:, kc, :bs], kc == 0, kc == 7, [bwo[oc // 4], byb], [bpso[p2]])
                    P.stt(xb[:, oc, :bs], pso[p2][:, :bs], MOD[:, 16 + oc, who:who + 1], xb[:, oc, :bs], ALU.mult, ALU.add, [bpso[p2], bMOD, bxb], [bxb])
                P.dma(XT_d[:, :, b0:b0 + bs], xb[:, :, :bs], reads=[bxb], writes=[bXT])
            P.barrier()
        if stop_after == "G":
            P.emit(); return nc

        with ES() as phm:
            rw = P.sb("Erw", [128, 8, 36], F32, phm); brw = Buf()
            P.dma(rw[:, :, 0:4], I["router_g_w"][l].rearrange("(k p) g -> p k g", p=128), writes=[brw], allow_slow_non_contiguous=True)
            P.dma(rw[:, :, 4:36], I["router_e_w"][l].rearrange("(k p) g -> p k g", p=128), writes=[brw], allow_slow_non_contiguous=True)
            rb = P.sb("Erb", [128, 36], F32, phm)
            P.dma(rb[:, 0:4], I["router_g_b"][l].partition_broadcast(128), writes=[brw]); P.dma(rb[:, 4:36], I["router_e_b"][l].partition_broadcast(128), writes=[brw])
            LG = P.sb("ELG", [128, 18, 36], F32, phm); bLG = Buf()
            WW = P.sb("EWW", [128, 18, 32], F32, phm); bWW = Buf()
            NTL = 18
            t0 = 0 if not last else 2
            norm_phase(1, qblocks, router=(rw, LG, bLG, brw))
            WT_d = nc.dram_tensor("WT_d%d" % l, [32, NT], F32, kind=dk).ap(); bWT = Buf()
            with ES() as ph:
                def tl(name, shp):
                    return P.sb(name, shp, F32, ph)
                gm = tl("Rgm", [128, 18]); geq = tl("Rgeq", [128, 18, 4]); gex = tl("Rgex", [128, 18, 4]); pg = tl("Rpg", [128, 18])
                elm = tl("Relm", [128, 18, 32]); m1 = tl("Rm1", [128, 18]); m2 = tl("Rm2", [128, 18]); s1 = tl("Rs1", [128, 18, 32]); s2 = tl("Rs2", [128, 18, 32])
                e2 = tl("Re2", [128, 18]); w1_ = tl("Rw1", [128, 18]); w2_ = tl("Rw2", [128, 18]); wts = tl("Rwts", [32, NT])
                bR = Buf()
                R_ = [bLG, bR, brw]
                ns = slice(t0, 18); nn = 18 - t0
                P.tt("dve", LG[:, ns], LG[:, ns], rb[:].unsqueeze(1).to_broadcast([128, nn, 36]), ALU.add, R_, [bLG])
                P.reduce(gm[:, ns], LG[:, ns, 0:4], ALU.max, R_, [bR])
                P.tt("dve", geq[:, ns], LG[:, ns, 0:4], gm[:, ns].unsqueeze(2).to_broadcast([128, nn, 4]), ALU.is_equal, R_, [bR])
                P.tt("dve", gex[:, ns], LG[:, ns, 0:4], gm[:, ns].unsqueeze(2).to_broadcast([128, nn, 4]), ALU.subtract, R_, [bR])
                P.act(gex[:, ns], gex[:, ns], AF.Exp, R_, [bR])
                P.reduce(pg[:, ns], gex[:, ns], ALU.add, R_, [bR])
                P.recip(pg[:, ns], pg[:, ns], R_, [bR])
                P.ts("dve", geq[:, ns], geq[:, ns], -1.0, 1e30, ALU.add, ALU.mult, reads=R_, writes=[bR])
                P.tt("dve", elm[:, ns].rearrange("p n (g e) -> p n g e", g=4), LG[:, ns, 4:36].rearrange("p n (g e) -> p n g e", g=4),
                     geq[:, ns].unsqueeze(3).to_broadcast([128, nn, 4, 8]), ALU.add, R_, [bR])
                P.reduce(m1[:, ns], elm[:, ns], ALU.max, R_, [bR])
                P.tt("dve", s1[:, ns], elm[:, ns], m1[:, ns].unsqueeze(2).to_broadcast([128, nn, 32]), ALU.is_equal, R_, [bR])
                P.stt(elm[:, ns], s1[:, ns], -1e30, elm[:, ns], ALU.mult, ALU.add, R_, [bR])
                P.reduce(m2[:, ns], elm[:, ns], ALU.max, R_, [bR])
                P.tt("dve", s2[:, ns], elm[:, ns], m2[:, ns].unsqueeze(2).to_broadcast([128, nn, 32]), ALU.is_equal, R_, [bR])
                P.tt("dve", e2[:, ns], m2[:, ns], m1[:, ns], ALU.subtract, R_, [bR])
                P.act(e2[:, ns], e2[:, ns], AF.Exp, R_, [bR])
                P.ts("dve", w1_[:, ns], e2[:, ns], 1.0, None, ALU.add, reads=R_, writes=[bR])
                P.recip(w1_[:, ns], w1_[:, ns], R_, [bR])
                P.tt("dve", w1_[:, ns], w1_[:, ns], pg[:, ns], ALU.mult, R_, [bR])
                P.tt("dve", w2_[:, ns], w1_[:, ns], e2[:, ns], ALU.mult, R_, [bR])
                P.tt("dve", s1[:, ns], s1[:, ns], w1_[:, ns].unsqueeze(2).to_broadcast([128, nn, 32]), ALU.mult, R_, [bR])
                P.tt("dve", s2[:, ns], s2[:, ns], w2_[:, ns].unsqueeze(2).to_broadcast([128, nn, 32]), ALU.mult, R_, [bR])
                P.tt("dve", WW[:, ns], s1[:, ns], s2[:, ns], ALU.add, R_, [bWW])
                ptw = P.ps("Rptw", [128, 512], F32, ph); bptw = Buf()
                for n in range(t0, 18):
                    P.tr(ptw[0:32, 0:128], WW[:, n, :], ident[:], [bWW, bconst], [bptw])
                    P.copy("dve", wts[:, n * 128:(n + 1) * 128], ptw[0:32, 0:128], [bptw], [bR])
                P.dma(WT_d[:, t0 * 128:NT], wts[:, t0 * 128:NT], reads=[bR], writes=[bWT])
                P.barrier()
            phx = ES(); phx.__enter__()
            xT = P.sb("ExT", [128, 8, NT], F32, phx); bx = Buf()
            with ES() as ph:
                hT = P.sb("EhT", [128, 8, NT], BF16, ph); bh = Buf()
                c_lo = qblocks[0][0]
                P.dma(hT[:, 0:4, c_lo:NT], HT_d[:, 0:4, c_lo:NT], reads=[bHT], writes=[bh]); P.dma(hT[:, 4:8, c_lo:NT], HT_d[:, 4:8, c_lo:NT], reads=[bHT], writes=[bh], q="pool")
                w1b = [P.sb("Ew1%d" % i, [128, 8, 512], BF16, ph) for i in range(2)]; w3b = [P.sb("Ew3%d" % i, [128, 8, 512], BF16, ph) for i in range(2)]
                w2b = P.sb("Ew2", [128, 4, 1024], BF16, ph)
                bw1 = [Buf(), Buf()]; bw3 = [Buf(), Buf()]; bw2 = Buf()
                stg = [P.sb("Estg%d" % i, [128, 2048], F32, ph) for i in range(2)]; bstg = [Buf(), Buf()]
                WB = [P.sb("EWB%d" % i, [128, 512], F32, ph) for i in range(2)]; bWB = [Buf(), Buf()]
                sgt = P.sb("Esg", [128, 512], F32, ph); t3 = P.sb("Et3", [128, 512], F32, ph); bsgt = Buf(); bt3 = Buf()
                gT = [P.sb("EgT%d" % i, [128, 4, 512], BF16, ph) for i in range(2)]; bgT = [Buf(), Buf()]
                ph1 = [P.ps("Eph1%d" % i, [128, 512], F32, ph) for i in range(2)]; bph1 = [Buf(), Buf()]
                ph3 = [P.ps("Eph3%d" % i, [128, 512], F32, ph) for i in range(2)]; bph3 = [Buf(), Buf()]
                pso = [P.ps("Epso%d" % i, [128, 512], F32, ph) for i in range(3)]; bpso = [Buf() for _ in range(3)]
                cnt = dict(ji=0, fi=0, oi=0)

                def loadw(src, dst, bdst, a_):
                    sv = src.rearrange("(k p) c -> p k c", p=128)
                    hk = a_ // 2
                    for half in range(2):
                        i2 = cnt["ji"] % 2; cnt["ji"] += 1
                        st_ = stg[i2][:].rearrange("p (a b) -> p a b", a=hk)
                        P.dma(st_, sv[:, half * hk:(half + 1) * hk, :], writes=[bstg[i2]], q="sp")
                        P.copy("act", dst[:, half * hk:(half + 1) * hk, :], st_, [bstg[i2]], [bdst])

                units = [(e, b0, bs) for e in range(32) for (b0, bs) in qblocks]
                nb = len(qblocks)

                def stage1(u):
                    e, b0, bs = units[u]
                    g_ = gT[u % 2]; bg_ = bgT[u % 2]
                    wb_ = WB[u % 2]; bwb_ = bWB[u % 2]
                    P.dma(wb_[:, :bs], WT_d[e, b0:b0 + bs].partition_broadcast(128), reads=[bWT], writes=[bwb_], q="pool")
                    for fc in range(4):
                        f2 = cnt["fi"] % 2; cnt["fi"] += 1
                        for kc in range(8):
                            P.mm(ph1[f2][:, :bs], w1b[e % 2][:, kc, fc * 128:(fc + 1) * 128], hT[:, kc, b0:b0 + bs], kc == 0, kc == 7, [bw1[e % 2], bh], [bph1[f2]])
                        for kc in range(8):
                            P.mm(ph3[f2][:, :bs], w3b[e % 2][:, kc, fc * 128:(fc + 1) * 128], hT[:, kc, b0:b0 + bs], kc == 0, kc == 7, [bw3[e % 2], bh], [bph3[f2]])
                        P.act(sgt[:, :bs], ph1[f2][:, :bs], AF.Silu, [bph1[f2]], [bsgt])
                        P.tt("dve", t3[:, :bs], ph3[f2][:, :bs], wb_[:, :bs], ALU.mult, [bph3[f2], bwb_], [bt3])
                        P.tt("pool", g_[:, fc, :bs], sgt[:, :bs], t3[:, :bs], ALU.mult, [bsgt, bt3], [bg_])

                def stage2(u):
                    e, b0, bs = units[u]
                    who = 1 if b0 < NCTX else 0
                    g_ = gT[u % 2]; bg_ = bgT[u % 2]
                    for oc in range(8):
                        o3 = cnt["oi"] % 3; cnt["oi"] += 1
                        for fc in range(4):
                            P.mm(pso[o3][:, :bs], w2b[:, fc, oc * 128:(oc + 1) * 128], g_[:, fc, :bs], fc == 0, fc == 3, [bw2, bg_], [bpso[o3]])
                        P.stt(xT[:, oc, b0:b0 + bs], pso[o3][:, :bs], MOD[:, 40 + oc, who:who + 1], xT[:, oc, b0:b0 + bs], ALU.mult, ALU.add, [bpso[o3], bMOD, bx], [bx])

                loadw(I["moe_w1"][l, 0], w1b[0], bw1[0], 8); loadw(I["moe_w3"][l, 0], w3b[0], bw3[0], 8); loadw(I["moe_w2"][l, 0], w2b, bw2, 4)
                for kc in range(8):
                    P.dma(xT[:, kc, c_lo:NT], XT_d[:, kc, c_lo:NT], reads=[bXT], writes=[bx], q="pool")
                for u in range(len(units) + 1):
                    if u < len(units):
                        e, b0, bs = units[u]
                        if u % nb == 0 and e + 1 < 32:
                            loadw(I["moe_w1"][l, e + 1], w1b[(e + 1) % 2], bw1[(e + 1) % 2], 8)
                            loadw(I["moe_w3"][l, e + 1], w3b[(e + 1) % 2], bw3[(e + 1) % 2], 8)
                        stage1(u)
                    if u >= 1:
                        stage2(u - 1)
                        e_, _, _ = units[u - 1]
                        if u % nb == 0 and e_ + 1 < 32:
                            loadw(I["moe_w2"][l, e_ + 1], w2b, bw2, 4)
                if not (last and layers == 2 and stop_after in (None, "ALL")):
                    for kc in range(8):
                        P.dma(XT_d[:, kc, c_lo:NT], xT[:, kc, c_lo:NT], reads=[bx], writes=[bXT], q="sp" if kc % 2 else "pool")
                P.barrier()
            if last and layers == 2 and stop_after in (None, "ALL"):
                final_phase(xT, bx)
            phx.__exit__(None, None, None)
        if stop_after == "E":
            P.emit(); return nc

    P.emit()
    return nc


_CACHE = {}


def make_in_maps(inputs, cores):
    consts = host_consts()
    maps = []
    for b in cores:
        m = {}
        for k, shp in IN_SHAPES.items():
            a = np.asarray(inputs[k], dtype=np.float32)
            if k in ("x", "ctx", "c"):
                a = a[b]
            m[k] = np.ascontiguousarray(a.reshape(shp))
        for k, v in consts.items():
            m["k_" + k] = np.ascontiguousarray(v)
        maps.append(m)
    return maps


def kernel(**inputs):
    if "nc" not in _CACHE:
        _CACHE["nc"] = build()
    nc = _CACHE["nc"]
    maps = make_in_maps(inputs, list(range(8)))
    res = run_bass_kernel_spmd(nc, maps, core_ids=list(range(8)))
    return np.stack([np.asarray(r["out"], dtype=np.float32) for r in res.results], axis=0)
```

```python
import contextlib
import numpy as np
import concourse.bass as bass
import concourse.mybir as mybir
from concourse.bass_utils import run_bass_kernel_spmd

F32 = mybir.dt.float32
BF16 = mybir.dt.bfloat16
I32 = mybir.dt.int32
AF = mybir.ActivationFunctionType
ALU = mybir.AluOpType
AX = mybir.AxisListType


class Buf:
    __slots__ = ("w", "r", "name")

    def __init__(self, name=""):
        self.w = None
        self.r = {}
        self.name = name


class Prog:
    ENG = ("pe", "act", "dve", "pool", "sp")

    def __init__(self, nc, ndma=12):
        self.nc = nc
        self.st = {e: [] for e in self.ENG}
        self.cnt = {e: 0 for e in self.ENG}
        self.seen = {e: {} for e in self.ENG}
        self.es = contextlib.ExitStack()
        self.sem = {}
        for e in ("pe", "act", "dve", "pool"):
            self.sem[("c", e)] = self.es.enter_context(nc.semaphore("s_" + e))
        self.ndma = ndma
        self.dcnt = {}
        self.dnext = {}
        for q in ("sp", "pool"):
            self.dnext[q] = 0
            for i in range(ndma):
                self.sem[("d", q, i)] = self.es.enter_context(nc.semaphore("d_%s%d" % (q, i)))
                self.dcnt[(q, i)] = 0
        self.nops = 0

    def sb(self, name, shape, dt, stack=None):
        self.uid = getattr(self, "uid", 0) + 1
        return (stack or self.es).enter_context(self.nc.sbuf_tensor("%s_%d" % (name, self.uid), list(shape), dt))

    def ps(self, name, shape, dt=F32, stack=None):
        self.uid = getattr(self, "uid", 0) + 1
        return (stack or self.es).enter_context(self.nc.psum_tensor("%s_%d" % (name, self.uid), list(shape), dt))

    def op(self, eng, fn, reads=(), writes=(), dma=False):
        deps = {}

        def add(tok):
            if tok is None:
                return
            k, v = tok
            if deps.get(k, 0) < v:
                deps[k] = v

        for b in reads:
            add(b.w)
        for b in writes:
            add(b.w)
            for k, v in b.r.items():
                add((k, v))
        if dma:
            q = eng
            i = self.dnext[q]
            self.dnext[q] = (i + 1) % self.ndma
            k = self.dcnt[(q, i)]
            if k > 0:
                add((("d", q, i), 16 * k))
            self.dcnt[(q, i)] = k + 1
            token = (("d", q, i), 16 * (k + 1))
        else:
            self.cnt[eng] += 1
            token = (("c", eng), self.cnt[eng])
        waits = []
        seen = self.seen[eng]
        for k, v in deps.items():
            if k == ("c", "pe") and eng == "pe":
                continue
            if seen.get(k, 0) >= v:
                continue
            seen[k] = v
            waits.append((k, v))
        self.st[eng].append((waits, fn, token, dma))
        for b in writes:
            b.w = token
            b.r = {}
        for b in reads:
            if b.r.get(token[0], 0) < token[1]:
                b.r[token[0]] = token[1]
        self.nops += 1
        return token

    def barrier(self):
        toks = []
        for e in ("pe", "act", "dve", "pool"):
            if self.cnt[e] > 0:
                toks.append((("c", e), self.cnt[e]))
        for q in ("sp", "pool"):
            for i in range(self.ndma):
                k = self.dcnt[(q, i)]
                if k > 0:
                    toks.append((("d", q, i), 16 * k))
        for e in self.ENG:
            waits = []
            seen = self.seen[e]
            for k, v in toks:
                if seen.get(k, 0) >= v:
                    continue
                seen[k] = v
                waits.append((k, v))
            if waits:
                self.st[e].append((waits, None, None, False))
        self.flush()

    def emit(self):
        self.barrier()

    def flush(self):
        if not any(self.st[e] for e in self.ENG):
            return
        nc = self.nc
        sem = self.sem
        st = self.st

        def run(e, eng):
            for waits, fn, token, dma in st[e]:
                for k, v in waits:
                    eng.wait_ge(sem[k], v)
                if fn is None:
                    continue
                ins = fn(eng)
                ins.then_inc(sem[token[0]], 16 if dma else 1)

        with nc.Block() as block:
            @block.tensor
            def _(eng):
                run("pe", eng)

            @block.scalar
            def _(eng):
                run("act", eng)

            @block.vector
            def _(eng):
                run("dve", eng)

            @block.gpsimd
            def _(eng):
                run("pool", eng)

            @block.sync
            def _(eng):
                run("sp", eng)
        self.st = {e: [] for e in self.ENG}

    def dma(self, out, in_, reads=(), writes=(), q="sp", **kw):
        return self.op(q, lambda e: e.dma_start(out=out, in_=in_, **kw), reads, writes, dma=True)

    def mm(self, out, lhsT, rhs, start, stop, reads=(), writes=(), **kw):
        return self.op("pe", lambda e: e.matmul(out, lhsT, rhs, start=start, stop=stop, **kw), reads, writes)

    def tr(self, out, in_, ident, reads=(), writes=()):
        return self.op("pe", lambda e: e.transpose(out, in_, ident), reads, writes)

    def act(self, out, in_, func, reads=(), writes=(), **kw):
        return self.op("act", lambda e: e.activation(out, in_, func, **kw), reads, writes)


def _sugar():
    def tt(self, eng, out, in0, in1, op, reads=(), writes=()):
        return self.op(eng, lambda e: e.tensor_tensor(out, in0, in1, op), reads, writes)

    def ts(self, eng, out, in0, s1, s2, op0, op1=None, reads=(), writes=(), **kw):
        if op1 is None:
            return self.op(eng, lambda e: e.tensor_scalar(out, in0, s1, None, op0, **kw), reads, writes)
        return self.op(eng, lambda e: e.tensor_scalar(out, in0, s1, s2, op0, op1, **kw), reads, writes)

    def stt(self, out, in0, scalar, in1, op0, op1, reads=(), writes=()):
        return self.op("dve", lambda e: e.scalar_tensor_tensor(out, in0, scalar, in1, op0, op1), reads, writes)

    def copy(self, eng, out, in_, reads=(), writes=()):
        if eng == "act":
            return self.op("act", lambda e: e.copy(out, in_), reads, writes)
        return self.op(eng, lambda e: e.tensor_copy(out, in_), reads, writes)

    def memset(self, eng, ap, val, writes=()):
        return self.op(eng, lambda e: e.memset(ap, val), (), writes)

    def recip(self, out, in_, reads=(), writes=()):
        return self.op("dve", lambda e: e.reciprocal(out, in_), reads, writes)

    def reduce(self, out, in_, op, reads=(), writes=(), axis=AX.X):
        return self.op("dve", lambda e: e.tensor_reduce(out, in_, axis, op), reads, writes)

    for f in (tt, ts, stt, copy, memset, recip, reduce):
        setattr(Prog, f.__name__, f)


_sugar()
import math
NT = 2304
NCTX = 256
NLAT = 2048
D = 1024
BLK = [(0, 256), (256, 512), (768, 512), (1280, 512), (1792, 512)]
EPS = 1e-6
CO = dict(Aq=0, Ai=512, Ag=1024, Aff=1536, Afb=2048, Bq=2560, Bk=3072, Bv=3584, Bo=4096, Bg=4608, Cq=4624, Ck=5136, Cv=5648)
FM = ["Aq", "Ag", "Aff", "Afb", "Bq", "Bk", "Bo", "Cq", "Ck"]
FMI = {n: 4 * i for i, n in enumerate(FM)}
TM = [("Ai", 0, 512), ("Bv", 512, 512), ("Cv", 1024, 512), ("Bg", 1536, 16)]
TMO = {n: o for n, o, w in TM}
NTM = 1552


class Ring:
    def __init__(self, items):
        self.items = items
        self.i = 0

    def next(self):
        x = self.items[self.i % len(self.items)]
        self.i += 1
        return x


def host_consts():
    c = {}
    c["ident"] = np.eye(128, dtype=np.float32)
    c["ones"] = np.ones((128, 128), np.float32)
    s = np.arange(128)[:, None]
    t = np.arange(128)[None, :]
    c["tri_f"] = (t >= s).astype(np.float32)
    c["tri_b"] = (t <= s).astype(np.float32)
    c["tri_fs"] = (t > s).astype(np.float32)
    s6 = np.arange(64)[:, None]; t6 = np.arange(64)[None, :]
    c["tri_bsw"] = ((((s6 + 32) % 64) >= t6)).astype(np.float32)
    tl = np.arange(NLAT)
    row = (tl // 64).astype(np.float32)
    col = (tl % 64).astype(np.float32)
    inv = (10000.0 ** (-np.arange(16, dtype=np.float32) / 16)).astype(np.float32)
    cosT = np.zeros((128, NLAT), np.float32)
    sinT = np.zeros((128, NLAT), np.float32)
    pm = np.zeros((128, 128), np.float32)
    for p in range(128):
        j = p % 32
        a = (p % 64) // 32
        i = j % 16
        pos = row if a == 0 else col
        ang = (pos * inv[i]).astype(np.float32)
        cosT[p] = np.cos(ang)
        sinT[p] = np.sin(ang)
        if j < 16:
            pm[p + 16, p] = -1.0
        else:
            pm[p - 16, p] = 1.0
    c["cosT"] = cosT
    c["sinT"] = sinT
    c["pm"] = pm
    sel = np.zeros((32, 32, 128), np.float32)
    for e in range(32):
        sel[e, e, :] = 1.0
    c["sel"] = sel.reshape(32, 32 * 128)
    m = np.ones((128, NT), np.float32)
    m[:, ::64] = 0.0
    c["rst64"] = m
    return c


CONST_SHAPES = dict(ident=[128, 128], ones=[128, 128], tri_f=[128, 128], tri_b=[128, 128], tri_fs=[128, 128], tri_bsw=[64, 64],
                    cosT=[128, NLAT], sinT=[128, NLAT], pm=[128, 128], sel=[32, 32 * 128], rst64=[128, NT])

IN_SHAPES = dict(
    x=[NLAT, D], ctx=[NCTX, D], c=[D], c_ctx=[D], ada_w=[2, D, 6 * D], ada_b=[2, 6 * D], norm1_g=[2, D], norm2_g=[2, D],
    w_in=[2, D, 6160], mlstm_conv_w=[2, 3, 1024], mlstm_conv_b=[2, 1024], mlstm_gate_b=[2, 16], hgrn_lb_raw=[2, 2, 512],
    hgrn_norm_g=[2, 512], mlstm_norm_g=[2, 512], diff_norm_g=[2, 512], diff_lambda=[2, 256], w_branch=[2, 3, 512, D],
    w_gate=[2, D, 3 * D], b_gate=[2, 3 * D], w_out=[2, D, D], router_g_w=[2, D, 4], router_g_b=[2, 4], router_e_w=[2, D, 32],
    router_e_b=[2, 32], moe_w1=[2, 32, D, 512], moe_w3=[2, 32, D, 512], moe_w2=[2, 32, 512, D], final_g=[D])


def build(stop_after=None, debug=False, layers=2):
    nc = bass.Bass("TRN2", target_bir_lowering=False)
    I = {k: nc.dram_tensor(k, s, F32, kind="ExternalInput").ap() for k, s in IN_SHAPES.items()}
    C = {k: nc.dram_tensor("k_" + k, s, F32, kind="ExternalInput").ap() for k, s in CONST_SHAPES.items()}
    OUT = nc.dram_tensor("out", [NLAT, D], F32, kind="ExternalOutput").ap()
    dk = "ExternalOutput" if debug else "Internal"
    XT_d = nc.dram_tensor("XT_d", [128, 8, NT], F32, kind=dk).ap()
    HT_d = nc.dram_tensor("HT_d", [128, 8, NT], BF16, kind=dk).ap()
    PF_d = nc.dram_tensor("PF_d", [36, 128, NT], F32, kind=dk).ap()
    PT_d = nc.dram_tensor("PT_d", [NT, NTM], F32, kind=dk).ap()
    Y_d = nc.dram_tensor("Y_d", [3, 128, 4, NT], BF16, kind=dk).ap()
    bXT, bHT, bPF, bPT, bY = Buf(), Buf(), [Buf() for _ in range(36)], Buf(), [Buf() for _ in range(3)]
    P = Prog(nc)
    ES = contextlib.ExitStack

    ident = P.sb("ident", [128, 128], F32); ones = P.sb("ones", [128, 128], F32)
    identb = P.sb("identb", [128, 128], BF16); onesb = P.sb("onesb", [128, 128], BF16)
    epsT = P.sb("epsT", [128, 1], F32)
    bconst = Buf()
    P.dma(ident[:], C["ident"], writes=[bconst]); P.dma(ones[:], C["ones"], writes=[bconst])
    P.copy("dve", identb[:], ident[:], [bconst], [bconst]); P.copy("dve", onesb[:], ones[:], [bconst], [bconst])
    P.memset("dve", epsT[:], EPS, [bconst])
    MOD = P.sb("MOD", [128, 48, 2], F32); bMOD = Buf()
    SC = P.sb("SC", [128, 2, 8, 2], F32)
    bSC = Buf()

    def vec_fm(dst, src, **kw):
        return P.dma(dst, src.rearrange("(k p) -> p k", p=128), allow_slow_non_contiguous=True, **kw)

    MODs = [MOD] + [P.sb("MOD%d" % i, [128, 48, 2], F32) for i in range(1, layers)]
    SCs = [SC] + [P.sb("SC%d" % i, [128, 2, 8, 2], F32) for i in range(1, layers)]
    with ES() as ph:
        stg = [P.sb("Lstg%d" % i, [128, D], F32, ph) for i in range(3)]; bstg = [Buf() for _ in range(3)]
        xo = [P.sb("Lxo%d" % i, [128, 8, 128], F32, ph) for i in range(2)]; bxo = [Buf() for _ in range(2)]
        pst = [P.ps("Lps%d" % i, [128, 512], F32, ph) for i in range(4)]; bps = [Buf() for _ in range(4)]
        cs = P.sb("Mcs", [128, 8, 2], F32, ph); bcs = Buf()
        vec_fm(cs[:, :, 0], I["c"], writes=[bcs]); vec_fm(cs[:, :, 1], I["c_ctx"], writes=[bcs])
        P.act(cs[:], cs[:], AF.Silu, [bcs], [bcs])
        ab = P.sb("Mab", [128, layers, 48], F32, ph); bab = Buf()
        ng = P.sb("Mng", [128, layers, 2, 8], F32, ph)
        for l_ in range(layers):
            vec_fm(ab[:, l_], I["ada_b"][l_], writes=[bab])
            vec_fm(ng[:, l_, 0, :], I["norm1_g"][l_], writes=[bab]); vec_fm(ng[:, l_, 1, :], I["norm2_g"][l_], writes=[bab])
        wst = [P.sb("Mw%d" % i, [128, 8, 768], F32, ph) for i in range(4)]; bw = [Buf() for _ in range(4)]
        pm_ = [P.ps("Mps%d" % i, [128, 512], F32, ph) for i in range(layers)]; bpm = [Buf() for _ in range(layers)]

        def L_step(t):
            src = I["ctx"][t * 128:(t + 1) * 128, :] if t < 2 else I["x"][(t - 2) * 128:(t - 1) * 128, :]
            s_ = t % 3
            P.dma(stg[s_][:], src, writes=[bstg[s_]], q="sp" if t % 2 == 0 else "pool")
            o = t % 2
            for half in range(2):
                pi = (2 * t + half) % 4
                for j in range(4):
                    kc = half * 4 + j
                    P.tr(pst[pi][:, j * 128:(j + 1) * 128], stg[s_][:, kc * 128:(kc + 1) * 128], ident[:], [bstg[s_], bconst], [bps[pi]])
                P.copy("dve" if half == 0 else "act", xo[o][:, half * 4:half * 4 + 4, :], pst[pi][:].rearrange("p (j c) -> p j c", j=4), [bps[pi]], [bxo[o]])
            P.dma(XT_d[:, :, t * 128:(t + 1) * 128], xo[o][:], reads=[bxo[o]], writes=[bXT])

        def M_group(l_, gI):
            k_ = (l_ * 8 + gI) % 4
            w = wst[k_]
            P.dma(w[:], I["ada_w"][l_, :, gI * 768:(gI + 1) * 768].rearrange("(k p) c -> p k c", p=128), writes=[bw[k_]], q="pool" if gI % 2 == 0 else "sp")
            for jj in range(6):
                j = gI * 6 + jj
                for kc in range(8):
                    P.mm(pm_[l_][:, 2 * j:2 * j + 2], w[:, kc, jj * 128:(jj + 1) * 128], cs[:, kc, :], kc == 0, kc == 7, [bw[k_], bcs], [bpm[l_]])

        def M_final(l_):
            MOD_ = MODs[l_]; SC_ = SCs[l_]
            P.tt("dve", MOD_[:], pm_[l_][:, 0:96].rearrange("p (j w) -> p j w", w=2), ab[:, l_].unsqueeze(2).to_broadcast([128, 48, 2]), ALU.add, [bpm[l_], bab], [bMOD])
            for n, sect in ((0, 1), (1, 4)):
                P.ts("dve", SC_[:, n], MOD_[:, sect * 8:(sect + 1) * 8, :], 1.0, None, ALU.add, reads=[bMOD], writes=[bSC])
                P.tt("dve", SC_[:, n], SC_[:, n], ng[:, l_, n, :].unsqueeze(2).to_broadcast([128, 8, 2]), ALU.mult, [bSC, bab], [bSC])

        mi = 0
        for t in range(18):
            L_step(t)
            if mi < layers * 8:
                M_group(mi // 8, mi % 8)
                if mi % 8 == 7:
                    M_final(mi // 8)
                mi += 1
        while mi < layers * 8:
            M_group(mi // 8, mi % 8)
            if mi % 8 == 7:
                M_final(mi // 8)
            mi += 1
        P.barrier()
    if stop_after == "L":
        P.emit(); return nc

    def final_phase(xres, bxres):
      with ES() as ph:
        fg = P.sb("Ffg", [128, 8], F32, ph); bfg = Buf()
        vec_fm(fg[:], I["final_g"], writes=[bfg])
        sq = P.sb("Fsq", [128, 8, 512], F32, ph); bsq = Buf()
        rs = P.sb("Frs", [128, 512], F32, ph); brs = Buf()
        ob = [P.sb("Fo%d" % i, [128, 1024], F32, ph) for i in range(2)]; bob = [Buf(), Buf()]
        pn = P.ps("Fpn", [128, 512], F32, ph); bpn = Buf()
        pt = [P.ps("Fpt%d" % i, [128, 512], F32, ph) for i in range(4)]; bpt = [Buf() for _ in range(4)]
        ti = 0
        for bi, (b0, bs) in enumerate(BLK[1:]):
            xv = xres[:, :, b0:b0 + bs]
            P.act(sq[:], xv, AF.Square, [bxres], [bsq])
            for kc in range(8):
                P.mm(pn[:], ones[:], sq[:, kc, :], kc == 0, kc == 7, [bsq, bconst], [bpn])
            P.act(rs[:], pn[:], AF.Sqrt, [bpn, bconst], [brs], bias=epsT[:], scale=1.0 / D)
            P.recip(rs[:], rs[:], [brs], [brs])
            P.tt("dve", sq[:], xv, rs[:].unsqueeze(1).to_broadcast([128, 8, 512]), ALU.mult, [bxres, brs], [bsq])
            for kc in range(8):
                P.act(sq[:, kc, :], sq[:, kc, :], AF.Copy, [bsq, bfg], [bsq], scale=fg[:, kc:kc + 1])
            for tt_ in range(4):
                o2 = ti % 2; ti += 1
                for half in range(2):
                    p4 = (2 * ti + half) % 4
                    for j in range(4):
                        kc = half * 4 + j
                        P.tr(pt[p4][:, j * 128:(j + 1) * 128], sq[:, kc, tt_ * 128:(tt_ + 1) * 128], ident[:], [bsq, bconst], [bpt[p4]])
                    P.copy("dve" if half == 0 else "act", ob[o2][:, half * 512:(half + 1) * 512], pt[p4][:], [bpt[p4]], [bob[o2]])
                r0 = b0 - NCTX + tt_ * 128
                P.dma(OUT[r0:r0 + 128, :], ob[o2][:], reads=[bob[o2]], q="sp" if tt_ % 2 else "pool")
        P.barrier()

    for l in range(layers):
        last = (l == 1)
        MOD = MODs[l]; SC = SCs[l]

        def norm_phase(n, blocks, router=None, into=None):
            shs = 0 if n == 0 else 3
            with ES() as ph_own:
                ph = ph_own if into is None else into[2]
                nsq = 2 if into is None else 1
                xb = [P.sb("Nx%d" % i, [128, 8, 512], F32, ph) for i in range(2)]; bxb = [Buf(), Buf()]
                sq_ = [P.sb("Nsq%d" % i, [128, 8, 512], F32, ph) for i in range(nsq)]; bsq_ = [Buf() for _ in range(nsq)]
                rs_ = [P.sb("Nrs%d" % i, [128, 512], F32, ph) for i in range(2)]; brs_ = [Buf(), Buf()]
                if into is None:
                    hb = [P.sb("Nh%d" % i, [128, 8, 512], BF16, ph) for i in range(2)]; bhb = [Buf(), Buf()]
                pn = [P.ps("Nps%d" % i, [128, 512], F32, ph) for i in range(2)]; bpn = [Buf(), Buf()]
                for bi, (b0, bs) in enumerate(blocks):
                    if into is not None:
                        i2 = bi % 2
                        sq = sq_[0]; bsq = bsq_[0]; rs = rs_[i2]; brs = brs_[i2]
                        who = 1 if b0 < NCTX else 0
                        hT_i, bh_i = into[0], into[1][bi]
                        P.dma(xb[i2][:, :, :bs], XT_d[:, :, b0:b0 + bs], reads=[bXT], writes=[bxb[i2]], q="sp" if i2 == 0 else "pool")
                        P.act(sq[:, :, :bs], xb[i2][:, :, :bs], AF.Square, [bxb[i2]], [bsq])
                        for kc in range(8):
                            P.mm(pn[i2][:, :bs], ones[:], sq[:, kc, :bs], kc == 0, kc == 7, [bsq, bconst], [bpn[i2]])
                        P.act(rs[:, :bs], pn[i2][:, :bs], AF.Sqrt, [bpn[i2], bconst], [brs], bias=epsT[:], scale=1.0 / D)
                        P.recip(rs[:, :bs], rs[:, :bs], [brs], [brs])
                        P.tt("dve", sq[:, :, :bs], xb[i2][:, :, :bs], rs[:, :bs].unsqueeze(1).to_broadcast([128, 8, bs]), ALU.mult, [bxb[i2], brs], [bsq])
                        for kc in range(8):
                            P.act(hT_i[:, kc, b0:b0 + bs], sq[:, kc, :bs], AF.Identity, [bsq, bSC, bMOD], [bh_i],
                                  scale=SC[:, n, kc, who:who + 1], bias=MOD[:, shs * 8 + kc, who:who + 1])
                        P.dma(HT_d[:, :, b0:b0 + bs], hT_i[:, :, b0:b0 + bs], reads=[bh_i], writes=[bHT], q="pool")
                        continue
                    who = 1 if b0 < NCTX else 0
                    i2 = bi % 2
                    sq = sq_[i2]; bsq = bsq_[i2]; rs = rs_[i2]; brs = brs_[i2]
                    P.dma(xb[i2][:, :, :bs], XT_d[:, :, b0:b0 + bs], reads=[bXT], writes=[bxb[i2]], q="sp" if i2 == 0 else "pool")
                    P.act(sq[:, :, :bs], xb[i2][:, :, :bs], AF.Square, [bxb[i2]], [bsq])
                    for kc in range(8):
                        P.mm(pn[i2][:, :bs], ones[:], sq[:, kc, :bs], kc == 0, kc == 7, [bsq, bconst], [bpn[i2]])
                    P.act(rs[:, :bs], pn[i2][:, :bs], AF.Sqrt, [bpn[i2], bconst], [brs], bias=epsT[:], scale=1.0 / D)
                    P.recip(rs[:, :bs], rs[:, :bs], [brs], [brs])
                    P.tt("dve", sq[:, :, :bs], xb[i2][:, :, :bs], rs[:, :bs].unsqueeze(1).to_broadcast([128, 8, bs]), ALU.mult, [bxb[i2], brs], [bsq])
                    if router is None:
                        for kc in range(8):
                            P.act(hb[i2][:, kc, :bs], sq[:, kc, :bs], AF.Identity, [bsq, bSC, bMOD], [bhb[i2]],
                                  scale=SC[:, n, kc, who:who + 1], bias=MOD[:, shs * 8 + kc, who:who + 1])
                    else:
                        rw_, LG_, bLG_, brw_ = router
                        for kc in range(8):
                            P.act(sq[:, kc, :bs], sq[:, kc, :bs], AF.Identity, [bsq, bSC, bMOD], [bsq],
                                  scale=SC[:, n, kc, who:who + 1], bias=MOD[:, shs * 8 + kc, who:who + 1])
                        P.copy("act", hb[i2][:, 0:4, :bs], sq[:, 0:4, :bs], [bsq], [bhb[i2]])
                        P.copy("dve", hb[i2][:, 4:8, :bs], sq[:, 4:8, :bs], [bsq], [bhb[i2]])
                        for tt_ in range(bs // 128):
                            tg = b0 // 128 + tt_
                            for kc in range(8):
                                P.mm(pn[i2][:, 0:36], sq[:, kc, tt_ * 128:(tt_ + 1) * 128], rw_[:, kc, :], kc == 0, kc == 7, [bsq, brw_], [bpn[i2]])
                            P.copy("dve", LG_[:, tg, :], pn[i2][:, 0:36], [bpn[i2]], [bLG_])
                    P.dma(HT_d[:, :, b0:b0 + bs], hb[i2][:, :, :bs], reads=[bhb[i2]], writes=[bHT])
                if into is None:
                    P.barrier()

        if stop_after == "N1":
            norm_phase(0, BLK)
            P.emit(); return nc

        with ES() as ph:
            hT = P.sb("PhT", [128, 8, NT], BF16, ph); bhs = [Buf() for _ in BLK]
            norm_phase(0, BLK, into=(hT, bhs, ph))

            def bh_of(c0, c1):
                return [bhs[i] for i, (b0, bs) in enumerate(BLK) if b0 < c1 and c0 < b0 + bs]
            wst = [P.sb("Pws%d" % i, [128, 8, 512], F32, ph) for i in range(2)]; bws = [Buf(), Buf()]
            wb = [P.sb("Pwb%d" % i, [128, 8, 512], BF16, ph) for i in range(2)]; bwb = [Buf(), Buf()]
            og = [P.sb("Pog%d" % i, [128, NT], F32, ph) for i in range(2)]; bog = [Buf(), Buf()]
            ot = [P.sb("Pot%d" % i, [128, 512], F32, ph) for i in range(3)]; bot = [Buf() for _ in range(3)]
            pp = [P.ps("Pps%d" % i, [128, 512], F32, ph) for i in range(4)]; bpp = [Buf() for _ in range(4)]
            gi = 0; ei = 0; oi = 0; ti = 0

            def loadw(c0, wdt):
                nonlocal gi
                i2 = gi % 2; gi += 1
                P.dma(wst[i2][:, :, :wdt], I["w_in"][l, :, c0:c0 + wdt].rearrange("(k p) c -> p k c", p=128), writes=[bws[i2]], q="sp" if i2 == 0 else "pool")
                P.copy("act" if i2 == 0 else "dve", wb[i2][:, :, :wdt], wst[i2][:, :, :wdt], [bws[i2]], [bwb[i2]])
                return wb[i2], bwb[i2]

            for name in FM:
                w, bw_ = loadw(CO[name], 512)
                for ch in range(4):
                    o2 = oi % 2; oi += 1
                    for (b0, bs) in BLK:
                        pi = ei % 4; ei += 1
                        for kc in range(8):
                            P.mm(pp[pi][:, :bs], w[:, kc, ch * 128:(ch + 1) * 128], hT[:, kc, b0:b0 + bs], kc == 0, kc == 7, [bw_] + bh_of(b0, b0 + bs), [bpp[pi]])
                        P.copy("act" if ei % 2 == 0 else "dve", og[o2][:, b0:b0 + bs], pp[pi][:, :bs], [bpp[pi]], [bog[o2]])
                    P.dma(PF_d[FMI[name] + ch], og[o2][:], reads=[bog[o2]], writes=[bPF[FMI[name] + ch]])
            for name, o0, wdt in TM:
                w, bw_ = loadw(CO[name], wdt)
                for t in range(18):
                    pi = ei % 4; ei += 1
                    for kc in range(8):
                        P.mm(pp[pi][:, :wdt], hT[:, kc, t * 128:(t + 1) * 128], w[:, kc, :wdt], kc == 0, kc == 7, [bw_] + bh_of(t * 128, (t + 1) * 128), [bpp[pi]])
                    t3 = ti % 3; ti += 1
                    P.copy("act" if ei % 2 == 0 else "dve", ot[t3][:, :wdt], pp[pi][:, :wdt], [bpp[pi]], [bot[t3]])
                    P.dma(PT_d[t * 128:(t + 1) * 128, o0:o0 + wdt], ot[t3][:, :wdt], reads=[bot[t3]], writes=[bPT], q="pool" if t % 2 else "sp")
            P.barrier()
        if stop_after == "P":
            P.emit(); return nc

        lam_init = 0.8 - 0.6 * math.exp(-0.3 * l)
        qblocks = (BLK if not last else BLK[1:])
        with ES() as ph:
            cosT = P.sb("Ccos", [128, NLAT], F32, ph); sinT = P.sb("Csin", [128, NLAT], F32, ph); pm = P.sb("Cpm", [128, 128], F32, ph)
            bcc = Buf()
            P.dma(cosT[:], C["cosT"], writes=[bcc]); P.dma(sinT[:], C["sinT"], writes=[bcc], q="pool"); P.dma(pm[:], C["pm"], writes=[bcc])
            lv = P.sb("Clv", [128, 256], F32, ph); lsc = P.sb("Clsc", [128, 4], F32, ph); nlam = P.sb("Cnlam", [128, 1], F32, ph); blam = Buf()
            P.dma(lv[:], I["diff_lambda"][l].partition_broadcast(128), writes=[blam])
            P.tt("dve", lv[:, 0:64], lv[:, 0:64], lv[:, 64:128], ALU.mult, [blam], [blam])
            P.tt("dve", lv[:, 128:192], lv[:, 128:192], lv[:, 192:256], ALU.mult, [blam], [blam])
            P.reduce(lsc[:, 0:1], lv[:, 0:64], ALU.add, [blam], [blam]); P.reduce(lsc[:, 1:2], lv[:, 128:192], ALU.add, [blam], [blam])
            P.act(lsc[:, 2:4], lsc[:, 0:2], AF.Exp, [blam], [blam])
            P.tt("dve", nlam[:], lsc[:, 3:4], lsc[:, 2:3], ALU.subtract, [blam], [blam])
            P.ts("dve", nlam[:], nlam[:], -lam_init, None, ALU.add, reads=[blam], writes=[blam])
            gcs = P.sb("Cg", [128, 4], F32, ph)
            vec_fm(gcs[:], I["diff_norm_g"][l], writes=[blam])
            P.ts("dve", gcs[:], gcs[:], 1.0 - lam_init, None, ALU.mult, reads=[blam], writes=[blam])
            V = P.sb("CV", [128, 18, 4, 129], BF16, ph); bV = Buf()
            P.memset("pool", V[:, :, :, 128:129], 1.0, [bV])
            vst = [P.sb("Cvst%d" % i, [128, 512], F32, ph) for i in range(2)]; bvst = [Buf(), Buf()]
            for t in range(18):
                P.dma(vst[t % 2][:], PT_d[t * 128:(t + 1) * 128, TMO["Cv"]:TMO["Cv"] + 512], reads=[bPT], writes=[bvst[t % 2]], q="sp" if t % 2 else "pool")
                P.copy("act" if t % 2 else "dve", V[:, t, :, 0:128], vst[t % 2][:].rearrange("p (h d) -> p h d", h=4), [bvst[t % 2]], [bV])
            q32_ = [P.sb("Cq32%d" % i, [128, NT], F32, ph) for i in range(2)]; k32_ = [P.sb("Ck32%d" % i, [128, NT], F32, ph) for i in range(2)]
            bq32_ = [Buf(), Buf()]; bk32_ = [Buf(), Buf()]
            qr_ = [P.sb("Cqr%d" % i, [128, NT], BF16, ph) for i in range(2)]; kr_ = [P.sb("Ckr%d" % i, [128, NT], BF16, ph) for i in range(2)]
            bqr_ = [Buf(), Buf()]; bkr_ = [Buf(), Buf()]
            t1 = P.sb("Ct1", [128, 512], F32, ph); t2 = P.sb("Ct2", [128, 512], F32, ph); bt1 = Buf(); bt2 = Buf()
            pTs = [[P.sb("CpT%d%d" % (c, i), [128, 512], BF16, ph) for i in range(2)] for c in range(2)]; bpT = [[Buf(), Buf()], [Buf(), Buf()]]
            oo_ = [P.sb("Coo%d" % i, [128, 4, 128], F32, ph) for i in range(2)]; boo_ = [Buf(), Buf()]
            tq = P.sb("Ctq", [128, 4, 128], F32, ph); btq = Buf()
            rc = P.sb("Crc", [128, 2, 4], F32, ph); ssq = P.sb("Cssq", [128, 4], F32, ph); brc = Buf()
            yst_ = [P.sb("Cyst%d" % i, [128, NT], BF16, ph) for i in range(2)]; byst_ = [Buf(), Buf()]
            posf = P.sb("Cposf", [128, 8, 129], F32, ph); bposf = Buf()
            ps_s = [[P.ps("Cps%d%d" % (c, i), [128, 512], F32, ph) for i in range(2)] for c in range(2)]; bps_s = [[Buf(), Buf()], [Buf(), Buf()]]
            pob = [P.ps("Cpob%d" % i, [128, 512], F32, ph) for i in range(3)]; bpob = [Buf() for _ in range(3)]
            ps_t = P.ps("Cpst", [128, 512], F32, ph); bps_t = Buf()
            ps_r = ps_s[0][0]; bps_r = bps_s[0][0]
            pend = []
            blkno = [0]
            ssq2_ = [P.sb("Cssq2%d" % i, [128, 4], F32, ph) for i in range(2)]; bssq2_ = [Buf(), Buf()]
            si = 0; pi_ = 0
            def prepC(h):
                q32 = q32_[h % 2]; k32 = k32_[h % 2]; bq32 = bq32_[h % 2]; bk32 = bk32_[h % 2]
                P.dma(q32[:], PF_d[FMI["Cq"] + h], reads=[bPF[FMI["Cq"] + h]], writes=[bq32])
                P.dma(k32[:], PF_d[FMI["Ck"] + h], reads=[bPF[FMI["Ck"] + h]], writes=[bk32], q="pool")
                for (src, bsrc, dst, bdst) in ((q32, bq32, qr_[h % 2], bqr_[h % 2]), (k32, bk32, kr_[h % 2], bkr_[h % 2])):
                    P.copy("pool", dst[:, 0:NCTX], src[:, 0:NCTX], [bsrc], [bdst])
                    for j in range(4):
                        c0 = NCTX + j * 512
                        P.mm(ps_t[:], pm[:], src[:, c0:c0 + 512], True, True, [bcc, bsrc], [bps_t])
                        P.tt("dve", t1[:], ps_t[:], sinT[:, j * 512:(j + 1) * 512], ALU.mult, [bps_t, bcc], [bt1])
                        P.tt("pool", t2[:], src[:, c0:c0 + 512], cosT[:, j * 512:(j + 1) * 512], ALU.mult, [bsrc, bcc], [bt2])
                        P.tt("dve", dst[:, c0:c0 + 512], t1[:], t2[:], ALU.add, [bt1, bt2], [bdst])

            prepC(0)
            prep_sched = []
            for h in range(4):
                if h + 1 < 4:
                    prep_sched.append((lambda hh: (lambda: prepC(hh)))(h + 1))
                qr = qr_[h % 2]; kr = kr_[h % 2]; bqr = bqr_[h % 2]; bkr = bkr_[h % 2]
                yst = yst_[h % 2]; byst = byst_[h % 2]
                for bix, (b0, bs) in enumerate(qblocks):
                    kts = list(range(2)) if b0 < NCTX else list(range(18))
                    nk = len(kts); nsub = bs // 128
                    oo = oo_[blkno[0] % 2]; boo = boo_[blkno[0] % 2]; blkno[0] += 1

                    def S_(ii):
                        kt = kts[ii]
                        for c in range(2):
                            P.mm(ps_s[c][ii % 2][:, :bs], kr[c * 64:(c + 1) * 64, kt * 128:(kt + 1) * 128], qr[c * 64:(c + 1) * 64, b0:b0 + bs], True, True, [bkr, bqr], [bps_s[c][ii % 2]])

                    S_(0)
                    started = set()
                    for ii, kt in enumerate(kts):
                        if ii + 1 < nk:
                            S_(ii + 1)
                        for c in range(2):
                            P.act(pTs[c][ii % 2][:, :bs], ps_s[c][ii % 2][:, :bs], AF.Exp, [bps_s[c][ii % 2]], [bpT[c][ii % 2]], scale=0.125)
                        for c in range(2):
                            for j in range(nsub):
                                f = c * 4 + j; bk = f // 3; off = (f % 3) * 129
                                st_ = (ii == 0 and bk not in started)
                                started.add(bk)
                                P.mm(pob[bk][:, off:off + 129], pTs[c][ii % 2][:, j * 128:(j + 1) * 128], V[:, kt, h, :], st_, ii == nk - 1,
                                     [bpT[c][ii % 2], bV], [bpob[bk]], skip_group_check=True)
                        if ii == min(9, nk - 1) and pend:
                            pend.pop(0)()
                        if ii == 13 and prep_sched:
                            prep_sched.pop(0)()
                    for c in range(2):
                        for j in range(nsub):
                            f = c * 4 + j; bk = f // 3; off = (f % 3) * 129
                            P.copy("dve", posf[:, f, :], pob[bk][:, off:off + 129], [bpob[bk]], [bposf])
                    for c in range(2):
                        P.recip(rc[:, c, :nsub], posf[:, 4 * c:4 * c + nsub, 128], [bposf], [brc])
                    P.ts("dve", rc[:, 1, :nsub], rc[:, 1, :nsub], nlam[:], None, ALU.mult, reads=[brc, blam], writes=[brc])
                    P.tt("dve", oo[:, :nsub], posf[:, 0:nsub, 0:128], rc[:, 0, :nsub].unsqueeze(2).to_broadcast([128, nsub, 128]), ALU.mult, [bposf, brc], [boo])
                    P.tt("dve", tq[:, :nsub], posf[:, 4:4 + nsub, 0:128], rc[:, 1, :nsub].unsqueeze(2).to_broadcast([128, nsub, 128]), ALU.mult, [bposf, brc], [btq])
                    P.tt("dve", oo[:, :nsub], oo[:, :nsub], tq[:, :nsub], ALU.add, [boo, btq], [boo])
                    P.tt("dve", tq[:, :nsub], oo[:, :nsub], oo[:, :nsub], ALU.mult, [boo], [btq])
                    P.reduce(ssq[:, :nsub], tq[:, :nsub], ALU.add, [btq], [brc])
                    ssq2 = ssq2_[blkno[0] % 2]; bssq2 = bssq2_[blkno[0] % 2]
                    P.copy("dve", ssq2[:, :nsub], ssq[:, :nsub], [brc], [bssq2])

                    def part2(oo=oo, boo=boo, nsub=nsub, b0=b0, bs=bs, h=h, yst=yst, byst=byst, lastblk=(bix == len(qblocks) - 1), ssq2=ssq2, bssq2=bssq2):
                        P.act(ssq2[:, :nsub], ssq2[:, :nsub], AF.Sqrt, [bssq2, bconst], [bssq2], bias=epsT[:], scale=1.0 / 128)
                        P.recip(ssq2[:, :nsub], ssq2[:, :nsub], [bssq2], [bssq2])
                        P.tt("dve", oo[:, :nsub], oo[:, :nsub], ssq2[:, :nsub].unsqueeze(2).to_broadcast([128, nsub, 128]), ALU.mult, [boo, bssq2], [boo])
                        for j in range(nsub):
                            P.tr(ps_t[:, j * 128:(j + 1) * 128], oo[:, j, :], ident[:], [boo, bconst], [bps_t])
                        P.ts("dve", yst[:, b0:b0 + bs], ps_t[:, :bs], gcs[:, h:h + 1], None, ALU.mult, reads=[bps_t, blam], writes=[byst])
                        if lastblk:
                            c_lo = qblocks[0][0]
                            P.dma(Y_d[2, :, h, c_lo:NT], yst[:, c_lo:NT], reads=[byst], writes=[bY[2]])

                    pend.append(part2)
            while pend:
                pend.pop(0)()
            P.barrier()
        if stop_after == "C":
            P.emit(); return nc

        with ES() as ph:
            rst = P.sb("Arst", [128, NT], F32, ph); bca = Buf()
            P.dma(rst[:], C["rst64"], writes=[bca])
            trm = [P.sb("Atri%d" % i, [64, 64], F32, ph) for i in range(2)]
            P.dma(trm[0][:], C["tri_f"][0:64, 0:64], writes=[bca]); P.dma(trm[1][:], C["tri_bsw"], writes=[bca])
            trmi = [P.sb("Atrii%d" % i, [64, 64], I32, ph) for i in range(2)]
            P.copy("dve", trmi[0][:], trm[0][:], [bca], [bca]); P.copy("dve", trmi[1][:], trm[1][:], [bca], [bca])
            lbr = P.sb("Albr", [128, 2, 2, 4], F32, ph); lb1 = P.sb("Alb1", [128, 2, 4], F32, ph); oml = P.sb("Aoml", [128, 2, 4], F32, ph)
            P.dma(lbr[:], I["hgrn_lb_raw"].rearrange("l d (h p) -> p l d h", p=128), writes=[bca], allow_slow_non_contiguous=True)
            P.tt("dve", lb1[:], lbr[:, 1], lbr[:, 0], ALU.subtract, [bca], [bca])
            P.act(lb1[:], lb1[:], AF.Sigmoid, [bca], [bca])
            P.ts("dve", oml[:], lb1[:], -1.0, 1.0, ALU.mult, ALU.add, reads=[bca], writes=[bca])
            ga = P.sb("Aga", [128, 4], F32, ph)
            vec_fm(ga[:], I["hgrn_norm_g"][l], writes=[bca])
            q32 = P.sb("Aq32", [128, NT], F32, ph); bq32 = Buf()
            T = [P.sb("AT%d" % i, [128, NT], F32, ph) for i in range(5)]; bT = [Buf() for _ in range(5)]
            qt = [P.sb("Aqt%d" % i, [128, NT], BF16, ph) for i in range(2)]; kt = [P.sb("Akt%d" % i, [128, NT], BF16, ph) for i in range(2)]
            qh = [P.sb("Aqh%d" % i, [128, NT], BF16, ph) for i in range(2)]; kh = [P.sb("Akh%d" % i, [128, NT], BF16, ph) for i in range(2)]
            bqk = [Buf(), Buf()]
            khT = [P.sb("AkhT%d" % i, [64, 36, 128], BF16, ph) for i in range(2)]; bkhT = [Buf(), Buf()]
            V64s = P.sb("AV64s", [64, 36, 128], F32, ph); V64b = P.sb("AV64b", [64, 36, 128], BF16, ph); bV64s = Buf(); bV64 = Buf()
            oacc = P.sb("Aoacc", [128, NT], F32, ph); boacc = Buf()
            V64sw = P.sb("AV64sw", [64, 36, 128], BF16, ph); bV64sw = Buf()
            bend = P.sb("Abend", [128, 36], F32, ph); bbend = Buf()
            dec = [P.sb("Adec%d" % i, [128, 36], F32, ph) for i in range(2)]; bdec = [Buf(), Buf()]
            S = [P.sb("AS%d" % i, [128, 128], F32, ph) for i in range(2)]; Sb = [P.sb("ASb%d" % i, [128, 128], BF16, ph) for i in range(2)]
            bS = [Buf(), Buf()]; bSb = [Buf(), Buf()]
            pT = [[P.sb("ApT%d%d" % (i, j), [64, 64], BF16, ph) for j in range(2)] for i in range(2)]; bpT = [[Buf(), Buf()], [Buf(), Buf()]]
            for i_ in range(2):
                for j_ in range(2):
                    P.memset("dve", pT[i_][j_][:], 0.0, [bpT[i_][j_]])
            rr_ = [P.sb("Arr%d" % i, [128, 512], F32, ph) for i in range(2)]; tq_ = [P.sb("Atq%d" % i, [128, 512], F32, ph) for i in range(2)]
            brr_ = [Buf(), Buf()]; btq_ = [Buf(), Buf()]; sgl_ = [P.sb("Asgl%d" % i, [128, 512], F32, ph) for i in range(2)]; bsgl_ = [Buf(), Buf()]
            yst = P.sb("Ayst", [128, NT], BF16, ph); byst = Buf()
            psc = [P.ps("Apsc%d" % i, [128, 512], F32, ph) for i in range(2)]; bpsc = [Buf(), Buf()]
            pso = [P.ps("Apso%d" % i, [128, 512], F32, ph) for i in range(2)]; bpso = [Buf(), Buf()]
            pst = [P.ps("Apst%d" % i, [128, 512], F32, ph) for i in range(2)]; bpst = [Buf(), Buf()]
            ptr = P.ps("Aptr", [128, 1024], BF16, ph); bptr = Buf()
            pn = P.ps("Apn", [128, 512], F32, ph); bpn = Buf()
            for i_ in range(2):
                P.memset("dve", psc[i_][:], 0.0, [bpsc[i_]])
            G = [(0, 4), (4, 12), (12, 20), (20, 28), (28, 36)]
            seqs = [[n for (g0, g1) in G for n in range(g0, g1)],
                    [n for (g0, g1) in [G[0], G[4], G[3], G[2], G[1]] for n in range(g1 - 1, g0 - 1, -1)]]
            grp = {n: g for g in G for n in range(g[0], g[1])}

            def v3(t_):
                return t_[:].rearrange("p (n j) -> p n j", j=64)

            for h in range(4):
                P.dma(q32[:], PF_d[FMI["Aq"] + h], reads=[bPF[FMI["Aq"] + h]], writes=[bq32])
                P.dma(V64s[:], PT_d[:, TMO["Ai"] + h * 128:TMO["Ai"] + (h + 1) * 128].rearrange("(c p) d -> p c d", p=64), reads=[bPT], writes=[bV64s], q="pool")
                P.copy("act", V64b[:, 0:18], V64s[:, 0:18], [bV64s], [bV64]); P.copy("dve", V64b[:, 18:36], V64s[:, 18:36], [bV64s], [bV64])
                vsrc = PT_d[:, TMO["Ai"] + h * 128:TMO["Ai"] + (h + 1) * 128].rearrange("(c h j) d -> h j c d", h=2, j=32)
                P.dma(V64s[0:32], vsrc[1], reads=[bPT], writes=[bV64s]); P.dma(V64s[32:64], vsrc[0], reads=[bPT], writes=[bV64s], q="pool")
                P.copy("act", V64sw[:, 0:18], V64s[:, 0:18], [bV64s], [bV64sw]); P.copy("dve", V64sw[:, 18:36], V64s[:, 18:36], [bV64s], [bV64sw])
                for d in range(2):
                    sgn = 1.0 if d == 0 else -1.0
                    zi = FMI["Aff" if d == 0 else "Afb"] + h
                    HV = [(0, 1152), (1152, 2304)]
                    bTh = [[Buf(), Buf()] for _ in range(5)]
                    bbh = [Buf(), Buf()]
                    for i_ in range(5):
                        for hf in range(2):
                            bTh[i_][hf].w = bT[i_].w; bTh[i_][hf].r = dict(bT[i_].r)
                    for hf in range(2):
                        bbh[hf].w = bbend.w; bbh[hf].r = dict(bbend.r)

                    def c3(t_, a, b):
                        return t_[:, a:b].rearrange("p (n j) -> p n j", j=64)

                    def c4(t_, a, b):
                        return t_[:, a:b].rearrange("p (n h j) -> p n h j", h=2, j=32)

                    def each(fn):
                        for hf, (a, b) in enumerate(HV):
                            fn(hf, a, b, slice(hf * 18, (hf + 1) * 18))

                    each(lambda hf, a, b, ns: P.dma(T[0][:, a:b], PF_d[zi][:, a:b], reads=[bPF[zi]], writes=[bTh[0][hf]], q="sp" if hf == 0 else "pool"))
                    each(lambda hf, a, b, ns: P.act(T[0][:, a:b], T[0][:, a:b], AF.Sigmoid, [bTh[0][hf]], [bTh[0][hf]]))
                    if l == 1:
                        each(lambda hf, a, b, ns: P.ts("dve", T[0][:, a:b], T[0][:, a:b], oml[:, d, h:h + 1], lb1[:, d, h:h + 1], ALU.mult, ALU.add, reads=[bTh[0][hf], bca], writes=[bTh[0][hf]]))
                    each(lambda hf, a, b, ns: P.act(T[1][:, a:b], T[0][:, a:b], AF.Ln, [bTh[0][hf]], [bTh[1][hf]]))
                    each(lambda hf, a, b, ns: P.ts("pool", T[2][:, a:b], T[0][:, a:b], -1.0, 1.0, ALU.mult, ALU.add, reads=[bTh[0][hf]], writes=[bTh[2][hf]]))
                    each(lambda hf, a, b, ns: P.op("dve", (lambda a=a, b=b: (lambda e: e.tensor_tensor_scan(T[3][:, a:b], rst[:, a:b], T[1][:, a:b], 0.0, ALU.mult, ALU.add)))(), [bca, bTh[1][hf]], [bTh[3][hf]]))
                    each(lambda hf, a, b, ns: P.copy("dve", bend[:, ns], c3(T[3], a, b)[:, :, 63], [bTh[3][hf]], [bbh[hf]]))
                    each(lambda hf, a, b, ns: P.act(dec[d][:, ns], bend[:, ns], AF.Exp, [bbh[hf]], [bdec[d]]))
                    if d == 1:
                        each(lambda hf, a, b, ns: P.tt("dve", T[3][:, a:b], T[3][:, a:b], T[1][:, a:b], ALU.subtract, [bTh[3][hf], bTh[1][hf]], [bTh[3][hf]]))
                    each(lambda hf, a, b, ns: P.tt("dve", c3(T[4], a, b), c3(T[3], a, b), c3(T[3], a, b)[:, :, 31:32].to_broadcast([128, 18, 64]), ALU.subtract, [bTh[3][hf]], [bTh[4][hf]]))
                    each(lambda hf, a, b, ns: P.act(T[1][:, a:b], T[4][:, a:b], AF.Exp, [bTh[4][hf]], [bTh[1][hf]], scale=sgn))
                    each(lambda hf, a, b, ns: P.tt("dve", qt[d][:, a:b], q32[:, a:b], T[1][:, a:b], ALU.mult, [bq32, bTh[1][hf]], [bqk[d]]))
                    each(lambda hf, a, b, ns: P.act(T[1][:, a:b], T[4][:, a:b], AF.Exp, [bTh[4][hf]], [bTh[1][hf]], scale=-sgn))

                    def kmul(dst):
                        def f(hf, a, b, ns):
                            if d == 0:
                                P.tt("dve", dst[:, a:b], T[2][:, a:b], T[1][:, a:b], ALU.mult, [bTh[2][hf], bTh[1][hf]], [bqk[d]])
                            else:
                                for hh in range(2):
                                    P.tt("dve", c4(dst, a, b)[:, :, 1 - hh, :], c4(T[2], a, b)[:, :, hh, :], c4(T[1], a, b)[:, :, hh, :], ALU.mult, [bTh[2][hf], bTh[1][hf]], [bqk[d]])
                        return f

                    def qmul(dst):
                        return lambda hf, a, b, ns: P.tt("dve", dst[:, a:b], q32[:, a:b], T[1][:, a:b], ALU.mult, [bq32, bTh[1][hf]], [bqk[d]])

                    each(kmul(kt[d]))
                    each(lambda hf, a, b, ns: P.act(T[1][:, a:b], T[3][:, a:b], AF.Exp, [bTh[3][hf]], [bTh[1][hf]]))
                    each(qmul(qh[d]) if d == 0 else kmul(kh[d]))
                    each(lambda hf, a, b, ns: P.tt("dve", c3(T[4], a, b), c3(T[3], a, b), bend[:, ns].unsqueeze(2).to_broadcast([128, 18, 64]), ALU.subtract, [bTh[3][hf], bbh[hf]], [bTh[4][hf]]))
                    each(lambda hf, a, b, ns: P.act(T[1][:, a:b], T[4][:, a:b], AF.Exp, [bTh[4][hf]], [bTh[1][hf]], scale=-1.0))
                    each(kmul(kh[d]) if d == 0 else qmul(qh[d]))
                    for i_ in range(5):
                        for hf in range(2):
                            bb = bTh[i_][hf]
                            if bb.w is not None and (bT[i_].w is None or True):
                                pass
                        mw = {}
                        for hf in range(2):
                            bb = bTh[i_][hf]
                            if bb.w is not None:
                                mw[bb.w[0]] = max(mw.get(bb.w[0], 0), bb.w[1])
                            for k_, v_ in bb.r.items():
                                mw[k_] = max(mw.get(k_, 0), v_)
                        bT[i_].w = None; bT[i_].r = mw
                    mw = {}
                    for hf in range(2):
                        if bbh[hf].w is not None:
                            mw[bbh[hf].w[0]] = max(mw.get(bbh[hf].w[0], 0), bbh[hf].w[1])
                        for k_, v_ in bbh[hf].r.items():
                            mw[k_] = max(mw.get(k_, 0), v_)
                    bbend.w = None; bbend.r = mw
                    for g8 in range(0, 36, 8):
                        ng = min(8, 36 - g8)
                        for j in range(ng):
                            n = g8 + j
                            P.tr(ptr[0:64, j * 128:(j + 1) * 128], kh[d][:, n * 64:(n + 1) * 64], identb[:], [bqk[d], bconst], [bptr])
                        P.copy("act", khT[d][:, g8:g8 + ng, :], ptr[0:64, 0:ng * 128].rearrange("p (n c) -> p n c", c=128), [bptr], [bkhT[d]])
                P.memset("pool", oacc[:], 0.0, [boacc])
                first = [True, True]
                for i in range(36):
                    for d in range(2):
                        n = seqs[d][i]
                        g0, g1 = grp[n]
                        cs_ = slice(n * 64, (n + 1) * 64)
                        sl = i % 8
                        c0 = sl * 64; n0 = n * 64
                        if d == 0:
                            P.mm(psc[d][0:32, c0:c0 + 32], kt[d][:, n0:n0 + 32], qt[d][:, n0:n0 + 32], True, True, [bqk[d]], [bpsc[d]])
                            P.mm(psc[d][0:64, c0 + 32:c0 + 64], kt[d][:, n0:n0 + 64], qt[d][:, n0 + 32:n0 + 64], True, True, [bqk[d]], [bpsc[d]])
                        else:
                            P.mm(psc[d][0:32, c0 + 32:c0 + 64], kt[d][:, n0:n0 + 32], qt[d][:, n0 + 32:n0 + 64], True, True, [bqk[d]], [bpsc[d]])
                            P.mm(psc[d][0:64, c0:c0 + 32], kt[d][:, n0:n0 + 64], qt[d][:, n0:n0 + 32], True, True, [bqk[d]], [bpsc[d]])
                        pt_ = pT[d][i % 2]; bpt_ = bpT[d][i % 2]
                        P.op("dve", (lambda o_, m_, d_: (lambda e: e.copy_predicated(o_, m_, d_)))(pt_[:], trmi[d][:], psc[d][0:64, sl * 64:(sl + 1) * 64]), [bpsc[d], bca, bpt_], [bpt_])
                        col = (n - g0) * 64
                        Vd = V64b if d == 0 else V64sw; bVd = bV64 if d == 0 else bV64sw
                        P.mm(pso[d][:, col:col + 64], Vd[:, n, :], pt_[:], True, first[d], [bVd, bpt_], [bpso[d]])
                        if not first[d]:
                            P.mm(pso[d][:, col:col + 64], Sb[d][:], qh[d][:, cs_], False, True, [bSb[d], bqk[d]], [bpso[d]])
                        ss = i % 4
                        P.mm(pst[d][:, ss * 128:(ss + 1) * 128], khT[d][:, n, :], Vd[:, n, :], True, True, [bkhT[d], bVd], [bpst[d]])
                        if first[d]:
                            P.copy("dve", S[d][:], pst[d][:, ss * 128:(ss + 1) * 128], [bpst[d]], [bS[d]])
                        else:
                            P.stt(S[d][:], S[d][:], dec[d][:, n:n + 1], pst[d][:, ss * 128:(ss + 1) * 128], ALU.mult, ALU.add, [bS[d], bdec[d], bpst[d]], [bS[d]])
                        P.copy("act", Sb[d][:], S[d][:], [bS[d]], [bSb[d]])
                        first[d] = False
                        lastn = (g1 - 1) if d == 0 else g0
                        if n == lastn:
                            w_ = (g1 - g0) * 64
                            P.tt("dve", oacc[:, g0 * 64:g1 * 64], pso[d][:, :w_], oacc[:, g0 * 64:g1 * 64], ALU.add, [bpso[d], boacc], [boacc])
                gi_ = FMI["Ag"] + h
                P.dma(T[0][:], PF_d[gi_], reads=[bPF[gi_]], writes=[bT[0]])
                for bi_, (b0, bs) in enumerate(qblocks):
                    rr = rr_[bi_ % 2]; brr = brr_[bi_ % 2]; tq = tq_[bi_ % 2]; btq = btq_[bi_ % 2]; sgl = sgl_[bi_ % 2]; bsgl = bsgl_[bi_ % 2]
                    P.act(tq[:, :bs], oacc[:, b0:b0 + bs], AF.Square, [boacc], [btq])
                    P.mm(pn[:, :bs], ones[:], tq[:, :bs], True, True, [bconst, btq], [bpn])
                    P.act(rr[:, :bs], pn[:, :bs], AF.Sqrt, [bpn, bconst], [brr], bias=epsT[:], scale=1.0 / 128)
                    P.recip(rr[:, :bs], rr[:, :bs], [brr], [brr])
                    P.tt("dve", tq[:, :bs], oacc[:, b0:b0 + bs], rr[:, :bs], ALU.mult, [boacc, brr], [btq])
                    P.act(sgl[:, :bs], T[0][:, b0:b0 + bs], AF.Silu, [bT[0]], [bsgl])
                    P.stt(yst[:, b0:b0 + bs], tq[:, :bs], ga[:, h:h + 1], sgl[:, :bs], ALU.mult, ALU.mult, [btq, bca, bsgl], [byst])
                c_lo = qblocks[0][0]
                P.dma(Y_d[0, :, h, c_lo:NT], yst[:, c_lo:NT], reads=[byst], writes=[bY[0]])
            P.barrier()
        if stop_after == "A":
            P.emit(); return nc

        with ES() as ph:
            bcb = Buf()
            trm = [P.sb("Btri%d" % i, [128, 128], F32, ph) for i in range(2)]
            P.dma(trm[0][:], C["tri_f"], writes=[bcb]); P.dma(trm[1][:], C["tri_b"], writes=[bcb])
            G_ = P.sb("BG", [128, 18, 16], F32, ph); gb = P.sb("Bgb", [128, 16], F32, ph); bG = Buf()
            P.dma(G_[:], PT_d[:, TMO["Bg"]:TMO["Bg"] + 16].rearrange("(n p) g -> p n g", p=128), reads=[bPT], writes=[bG], allow_slow_non_contiguous=True)
            P.dma(gb[:], I["mlstm_gate_b"][l].partition_broadcast(128), writes=[bcb])
            P.tt("dve", G_[:], G_[:], gb[:].unsqueeze(1).to_broadcast([128, 18, 16]), ALU.add, [bG, bcb], [bG])
            LF = P.sb("BLF", [128, 18, 8], F32, ph); LI = P.sb("BLI", [128, 18, 8], F32, ph); bLF = Buf()
            P.act(LF[:], G_[:, :, 8:16], AF.Sigmoid, [bG], [bLF])
            P.act(LF[:], LF[:], AF.Ln, [bLF], [bLF])
            P.copy("dve", LI[:], G_[:, :, 0:8], [bG], [bLF])
            GI = P.sb("BGI", [128, 18, 8], F32, ph); TT = P.sb("BTT", [128, 18, 8], F32, ph)
            A_ = P.sb("BA", [128, 18, 8], F32, ph); Cc = P.sb("BC", [128, 18, 8], F32, ph); Ee = P.sb("BE", [128, 18, 8], F32, ph); DT = P.sb("BDT", [128, 18, 8], F32, ph)
            bsc = Buf()
            psS = [P.ps("BpsS%d" % i, [128, 512], F32, ph) for i in range(2)]; bpsS = [Buf(), Buf()]
            psH = [P.ps("BpsH%d" % i, [128, 512], F32, ph) for i in range(2)]; bpsH = [Buf(), Buf()]
            psC = [P.ps("BpsC%d" % i, [128, 512], F32, ph) for i in range(2)]; bpsC = [Buf(), Buf()]
            pkt = P.ps("Bpkt", [128, 1024], BF16, ph); bpkt = Buf()
            pfin = P.ps("Bpfin", [128, 512], F32, ph); bpfin = Buf()
            LFf = LF[:].rearrange("p n j -> p (n j)")
            P.mm(psS[0][:, 0:144], trm[0][:], LFf, True, True, [bcb, bLF], [bpsS[0]])
            P.mm(psS[1][:, 0:144], ones[:], LFf, True, True, [bconst, bLF], [bpsS[1]])
            P.copy("dve", GI[:].rearrange("p n j -> p (n j)"), psS[0][:, 0:144], [bpsS[0]], [bsc])
            P.copy("dve", TT[:].rearrange("p n j -> p (n j)"), psS[1][:, 0:144], [bpsS[1]], [bsc])
            P.copy("dve", A_[:, :, 0:4], GI[:, :, 0:4], [bsc], [bsc])
            P.tt("dve", A_[:, :, 4:8], TT[:, :, 4:8], GI[:, :, 4:8], ALU.subtract, [bsc], [bsc])
            P.tt("dve", A_[:, :, 4:8], A_[:, :, 4:8], LF[:, :, 4:8], ALU.add, [bsc, bLF], [bsc])
            P.tt("dve", Cc[:], LI[:], A_[:], ALU.subtract, [bsc, bLF], [bsc])
            P.act(Cc[:], Cc[:], AF.Exp, [bsc], [bsc])
            P.tt("dve", Ee[:, :, 0:4], TT[:, :, 0:4], GI[:, :, 0:4], ALU.subtract, [bsc], [bsc])
            P.tt("dve", Ee[:, :, 4:8], GI[:, :, 4:8], LF[:, :, 4:8], ALU.subtract, [bsc, bLF], [bsc])
            P.tt("dve", Ee[:], Ee[:], LI[:], ALU.add, [bsc, bLF], [bsc])
            P.act(Ee[:], Ee[:], AF.Exp, [bsc], [bsc])
            P.act(DT[:], TT[:], AF.Exp, [bsc], [bsc])
            P.act(A_[:], A_[:], AF.Exp, [bsc], [bsc])
            cw = P.sb("Bcw", [128, 3, 8], F32, ph); cb = P.sb("Bcb", [128, 8], F32, ph)
            P.dma(cw[:], I["mlstm_conv_w"][l].rearrange("j (c p) -> p j c", p=128), writes=[bcb], allow_slow_non_contiguous=True)
            vec_fm(cb[:], I["mlstm_conv_b"][l], writes=[bcb])
            gB = P.sb("BgB", [128, 512], F32, ph)
            P.dma(gB[:], I["mlstm_norm_g"][l].partition_broadcast(128), writes=[bcb])
            V1 = P.sb("BV1", [128, 18, 4, 129], BF16, ph); bV1 = Buf()
            P.memset("pool", V1[:, :, :, 128:129], 1.0, [bV1])
            vst = [P.sb("Bvst%d" % i, [128, 512], F32, ph) for i in range(2)]; bvst = [Buf(), Buf()]
            for t in range(18):
                P.dma(vst[t % 2][:], PT_d[t * 128:(t + 1) * 128, TMO["Bv"]:TMO["Bv"] + 512], reads=[bPT], writes=[bvst[t % 2]], q="sp" if t % 2 else "pool")
                P.copy("act" if t % 2 else "dve", V1[:, t, :, 0:128], vst[t % 2][:].rearrange("p (h d) -> p h d", h=4), [bvst[t % 2]], [bV1])
            x32 = [P.sb("Bx%d" % i, [128, NT], F32, ph) for i in range(2)]; bx32 = [Buf(), Buf()]
            cv = [P.sb("Bcv%d" % i, [128, NT], F32, ph) for i in range(2)]; bcv = [Buf(), Buf()]
            qT = P.sb("BqT", [128, NT], BF16, ph); kT = P.sb("BkT", [128, NT], BF16, ph); bqT = Buf(); bkT = Buf()
            ktm = [P.sb("Bktm%d" % i, [128, 18, 128], BF16, ph) for i in range(2)]; bktm = [Buf(), Buf()]
            hacc = P.sb("Bhacc", [128, 18, 512], F32, ph); bhacc = Buf()
            P.memset("pool", hacc[:], 0.0, [bhacc])
            CN = [P.sb("BCN%d" % i, [128, 129], F32, ph) for i in range(2)]; CNb = [P.sb("BCNb%d" % i, [128, 129], BF16, ph) for i in range(2)]
            bCN = [Buf(), Buf()]; bCNb = [Buf(), Buf()]
            pT = [[P.sb("BpT%d%d" % (i, j), [128, 128], BF16, ph) for j in range(2)] for i in range(2)]; bpT = [[Buf(), Buf()], [Buf(), Buf()]]
            ND = P.sb("BND", [128, 18, 2, 129], F32, ph); bND = Buf()
            rd = P.sb("Brd", [128, 18, 2], F32, ph); rd2 = P.sb("Brd2", [128, 18, 2], F32, ph)
            CNb2 = [[P.sb("BCNb%d%d" % (i, j), [128, 129], BF16, ph) for j in range(2)] for i in range(2)]; bCNb2 = [[Buf(), Buf()], [Buf(), Buf()]]
            seqs = [list(range(18)), [1, 0] + list(range(17, 1, -1))]
            for h in range(4):
                for w_, (nm, cc) in enumerate((("Bq", h), ("Bk", 4 + h))):
                    xi = FMI[nm] + h
                    x_ = x32[w_]; o_ = cv[w_]
                    P.dma(x_[:], PF_d[xi], reads=[bPF[xi]], writes=[bx32[w_]], q="sp" if w_ == 0 else "pool")
                    P.ts("dve", o_[:], x_[:], cw[:, 1, cc:cc + 1], cb[:, cc:cc + 1], ALU.mult, ALU.add, reads=[bx32[w_], bcb], writes=[bcv[w_]])
                    for (a, b) in ((0, NCTX), (NCTX, NT)):
                        P.stt(o_[:, a + 1:b], x_[:, a:b - 1], cw[:, 0, cc:cc + 1], o_[:, a + 1:b], ALU.mult, ALU.add, [bx32[w_], bcb, bcv[w_]], [bcv[w_]])
                        P.stt(o_[:, a:b - 1], x_[:, a + 1:b], cw[:, 2, cc:cc + 1], o_[:, a:b - 1], ALU.mult, ALU.add, [bx32[w_], bcb, bcv[w_]], [bcv[w_]])
                    if w_ == 0:
                        P.act(o_[:], o_[:], AF.Silu, [bcv[w_]], [bcv[w_]])
                        P.ts("dve", qT[:], o_[:], 128.0 ** -0.5, None, ALU.mult, reads=[bcv[w_]], writes=[bqT])
                    else:
                        P.act(kT[:], o_[:], AF.Silu, [bcv[w_]], [bkT])
                for n in range(18):
                    sl = n % 8
                    P.tr(pkt[:, sl * 128:(sl + 1) * 128], kT[:, n * 128:(n + 1) * 128], identb[:], [bkT, bconst], [bpkt])
                    P.act(ktm[0][:, n, :], pkt[:, sl * 128:(sl + 1) * 128], AF.Copy, [bpkt, bsc], [bktm[0]], scale=Ee[:, n, h:h + 1])
                    P.ts("dve", ktm[1][:, n, :], pkt[:, sl * 128:(sl + 1) * 128], Ee[:, n, 4 + h:5 + h], None, ALU.mult, reads=[bpkt, bsc], writes=[bktm[1]])
                first = [True, True]
                for i in range(18):
                    for d in range(2):
                        n = seqs[d][i]; j = d * 4 + h
                        ts_ = slice(n * 128, (n + 1) * 128)
                        sl = i % 4
                        pss = psS[d][:, sl * 128:(sl + 1) * 128]
                        P.mm(pss, kT[:, ts_], qT[:, ts_], True, True, [bkT, bqT], [bpsS[d]])
                        pt_ = pT[d][i % 2]; bpt_ = bpT[d][i % 2]
                        P.stt(pt_[:], pss, Cc[:, n, j:j + 1], trm[d][:], ALU.mult, ALU.mult, [bpsS[d], bsc, bcb], [bpt_])
                        P.mm(psH[d][:, 0:129], pt_[:], V1[:, n, h, :], True, first[d], [bpt_, bV1], [bpsH[d]])
                        if not first[d]:
                            P.mm(psH[d][:, 0:129], qT[:, ts_], CNb2[d][(i + 1) % 2][:], False, True, [bqT, bCNb2[d][(i + 1) % 2]], [bpsH[d]])
                        P.copy("act", ND[:, n, d, :], psH[d][:, 0:129], [bpsH[d]], [bND])
                        P.mm(psC[d][:, 0:129], ktm[d][:, n, :], V1[:, n, h, :], True, True, [bktm[d], bV1], [bpsC[d]])
                        if first[d]:
                            P.copy("dve", CN[d][:], psC[d][:, 0:129], [bpsC[d]], [bCN[d]])
                        else:
                            P.stt(CN[d][:], CN[d][:], DT[:, n, j:j + 1], psC[d][:, 0:129], ALU.mult, ALU.add, [bCN[d], bsc, bpsC[d]], [bCN[d]])
                        P.copy("act", CNb2[d][i % 2][:], CN[d][:], [bCN[d]], [bCNb2[d][i % 2]])
                        first[d] = False
                av = A_[:, :, h:h + 5:4]
                P.tt("dve", ND[:], ND[:], av.unsqueeze(3).to_broadcast([128, 18, 2, 129]), ALU.mult, [bND, bsc], [bND])
                P.ts("dve", rd[:], ND[:, :, :, 128], -1.0, 1.0, ALU.mult, ALU.max, reads=[bND], writes=[bND])
                P.ts("dve", rd2[:], ND[:, :, :, 128], 1.0, None, ALU.max, reads=[bND], writes=[bND])
                P.tt("dve", rd[:], rd[:], rd2[:], ALU.max, [bND], [bND])
                P.recip(rd[:], rd[:], [bND], [bND])
                P.tt("dve", ND[:, :, :, 0:128], ND[:, :, :, 0:128], rd[:].unsqueeze(3).to_broadcast([128, 18, 2, 128]), ALU.mult, [bND], [bND])
                P.tt("dve", hacc[:, :, h * 128:(h + 1) * 128], ND[:, :, 0, 0:128], ND[:, :, 1, 0:128], ALU.add, [bND], [bhacc])
            sq = P.sb("Bsq", [128, 18, 512], F32, ph); bsq = Buf()
            ss = P.sb("Bss", [128, 72], F32, ph)
            P.tt("dve", sq[:], hacc[:], hacc[:], ALU.mult, [bhacc], [bsq])
            P.reduce(ss[:], sq[:].rearrange("p n (h d) -> p (n h) d", h=4), ALU.add, [bsq], [bsq])
            P.act(ss[:], ss[:], AF.Sqrt, [bsq, bconst], [bsq], bias=epsT[:], scale=1.0 / 128)
            P.recip(ss[:], ss[:], [bsq], [bsq])
            hv = hacc[:].rearrange("p n (h d) -> p (n h) d", h=4)
            P.tt("dve", hv, hv, ss[:].unsqueeze(2).to_broadcast([128, 72, 128]), ALU.mult, [bhacc, bsq], [bhacc])
            P.tt("dve", hacc[:], hacc[:], gB[:].unsqueeze(1).to_broadcast([128, 18, 512]), ALU.mult, [bhacc, bcb], [bhacc])
            yst = P.sb("Byst", [128, NT], BF16, ph); byst = Buf()
            for h in range(4):
                oi_ = FMI["Bo"] + h
                P.dma(x32[0][:], PF_d[oi_], reads=[bPF[oi_]], writes=[bx32[0]])
                P.act(x32[0][:], x32[0][:], AF.Sigmoid, [bx32[0]], [bx32[0]])
                for g4 in range(0, 18, 4):
                    ng = min(4, 18 - g4)
                    for jj in range(ng):
                        P.tr(pfin[:, jj * 128:(jj + 1) * 128], hacc[:, g4 + jj, h * 128:(h + 1) * 128], ident[:], [bhacc, bconst], [bpfin])
                    cs_ = slice(g4 * 128, (g4 + ng) * 128)
                    P.tt("dve", yst[:, cs_], pfin[:, 0:ng * 128], x32[0][:, cs_], ALU.mult, [bpfin, bx32[0]], [byst])
                c_lo = qblocks[0][0]
                P.dma(Y_d[1, :, h, c_lo:NT], yst[:, c_lo:NT], reads=[byst], writes=[bY[1]])
            P.barrier()
        if stop_after == "B":
            P.emit(); return nc

        with ES() as ph:
            wg = P.sb("Gwg", [128, 8, 3072], BF16, ph); wbr = P.sb("Gwbr", [128, 12, 1024], BF16, ph); wo = P.sb("Gwo", [128, 8, 1024], BF16, ph)
            bwts = Buf()
            bgt = P.sb("Gbg", [128, 24], F32, ph)
            vec_fm(bgt[:], I["b_gate"][l], writes=[bwts])
            stg = [P.sb("Gstg%d" % i, [128, 4096], F32, ph) for i in range(2)]; bstg = [Buf(), Buf()]
            bwg = [Buf() for _ in range(6)]; bwbr = [Buf() for _ in range(3)]; bwo = [Buf() for _ in range(2)]
            jobs = []

            def jg(g6):
                jobs.append((I["w_gate"][l, :, g6 * 512:(g6 + 1) * 512].rearrange("(k p) c -> p k c", p=128), 8, 512, wg[:, :, g6 * 512:(g6 + 1) * 512], bwg[g6]))

            def jb(m):
                jobs.append((I["w_branch"][l, m].rearrange("(k p) c -> p k c", p=128), 4, 1024, wbr[:, m * 4:(m + 1) * 4, :], bwbr[m]))

            jg(0); jb(0); jg(2); jb(1); jg(4); jb(2); jg(1); jg(3); jg(5)
            for g2_ in range(2):
                jobs.append((I["w_out"][l, :, g2_ * 512:(g2_ + 1) * 512].rearrange("(k p) c -> p k c", p=128), 8, 512, wo[:, :, g2_ * 512:(g2_ + 1) * 512], bwo[g2_]))
            for ji, (src, a_, b_, dst, bdst) in enumerate(jobs):
                i2 = ji % 2
                sv = stg[i2][:].rearrange("p (a b) -> p a b", a=a_)
                P.dma(sv, src, writes=[bstg[i2]], q="sp" if i2 == 0 else "pool")
                P.copy(("dve", "act")[ji % 2], dst, sv, [bstg[i2]], [bdst])
            hb = P.sb("Ghb", [128, 8, 512], BF16, ph); bhb = Buf()
            Yb = P.sb("GYb", [128, 3, 4, 512], BF16, ph); bYb = Buf()
            xb = P.sb("Gxb", [128, 8, 512], F32, ph); bxb = Buf()
            yb_ = P.sb("Gy", [128, 8, 512], BF16, ph); byb = Buf()
            sg = [P.sb("Gsg%d" % i, [128, 512], F32, ph) for i in range(2)]; bsg = [Buf(), Buf()]
            yacc = P.sb("Gyacc", [128, 512], F32, ph); byacc = Buf()
            tmp = P.sb("Gtmp", [128, 512], F32, ph); btmp = Buf()
            psg = [P.ps("Gpsg%d" % i, [128, 512], F32, ph) for i in range(3)]; bpsg = [Buf() for _ in range(3)]
            psb = [P.ps("Gpsb%d" % i, [128, 512], F32, ph) for i in range(3)]; bpsb = [Buf() for _ in range(3)]
            pso = [P.ps("Gpso%d" % i, [128, 512], F32, ph) for i in range(2)]; bpso = [Buf(), Buf()]
            gi = 0
            for (b0, bs) in qblocks:
                who = 1 if b0 < NCTX else 0
                P.dma(hb[:, :, :bs], HT_d[:, :, b0:b0 + bs], reads=[bHT], writes=[bhb])
                for m in range(3):
                    P.dma(Yb[:, m, :, :bs], Y_d[m, :, :, b0:b0 + bs], reads=[bY[m]], writes=[bYb], q="pool")
                P.dma(xb[:, :, :bs], XT_d[:, :, b0:b0 + bs], reads=[bXT], writes=[bxb])
                for oc in range(8):
                    for m in range(3):
                        pi = gi % 3; gi += 1
                        for kc in range(8):
                            P.mm(psg[pi][:, :bs], wg[:, kc, m * 1024 + oc * 128:m * 1024 + (oc + 1) * 128], hb[:, kc, :bs], kc == 0, kc == 7, [bwg[m * 2 + oc // 4], bhb], [bpsg[pi]])
                        for kc in range(4):
                            P.mm(psb[pi][:, :bs], wbr[:, m * 4 + kc, oc * 128:(oc + 1) * 128], Yb[:, m, kc, :bs], kc == 0, kc == 3, [bwbr[m], bYb], [bpsb[pi]])
                        s2 = gi % 2
                        P.act(sg[s2][:, :bs], psg[pi][:, :bs], AF.Sigmoid, [bpsg[pi], bwts], [bsg[s2]], bias=bgt[:, m * 8 + oc:m * 8 + oc + 1])
                        if m == 0:
                            P.tt("dve", yacc[:, :bs], sg[s2][:, :bs], psb[pi][:, :bs], ALU.mult, [bsg[s2], bpsb[pi]], [byacc])
                        else:
                            P.tt("dve", tmp[:, :bs], sg[s2][:, :bs], psb[pi][:, :bs], ALU.mult, [bsg[s2], bpsb[pi]], [btmp])
                            if m == 1:
                                P.tt("pool", yacc[:, :bs], yacc[:, :bs], tmp[:, :bs], ALU.add, [byacc, btmp], [byacc])
                            else:
                                P.tt("pool", yb_[:, oc, :bs], yacc[:, :bs], tmp[:, :bs], ALU.add, [byacc, btmp], [byb])
                for oc in range(8):
                    p2 = oc % 2
                    for kc in range(8):
                        P.mm(pso[p2][:, :bs], wo[:, kc, oc * 128:(oc + 1) * 128], yb_[:, kc, :bs], kc == 0, kc == 7, [bwo[oc // 4], byb], [bpso[p2]])
                    P.stt(xb[:, oc, :bs], pso[p2][:, :bs], MOD[:, 16 + oc, who:who + 1], xb[:, oc, :bs], ALU.mult, ALU.add, [bpso[p2], bMOD, bxb], [bxb])
                P.dma(XT_d[:, :, b0:b0 + bs], xb[:, :, :bs], reads=[bxb], writes=[bXT])
            P.barrier()
        if stop_after == "G":
            P.emit(); return nc

        with ES() as phm:
            rw = P.sb("Erw", [128, 8, 36], F32, phm); brw = Buf()
            P.dma(rw[:, :, 0:4], I["router_g_w"][l].rearrange("(k p) g -> p k g", p=128), writes=[brw], allow_slow_non_contiguous=True)
            P.dma(rw[:, :, 4:36], I["router_e_w"][l].rearrange("(k p) g -> p k g", p=128), writes=[brw], allow_slow_non_contiguous=True)
            rb = P.sb("Erb", [128, 36], F32, phm)
            P.dma(rb[:, 0:4], I["router_g_b"][l].partition_broadcast(128), writes=[brw]); P.dma(rb[:, 4:36], I["router_e_b"][l].partition_broadcast(128), writes=[brw])
            LG = P.sb("ELG", [128, 18, 36], F32, phm); bLG = Buf()
            WW = P.sb("EWW", [128, 18, 32], F32, phm); bWW = Buf()
            NTL = 18
            t0 = 0 if not last else 2
            norm_phase(1, qblocks, router=(rw, LG, bLG, brw))
            WT_d = nc.dram_tensor("WT_d%d" % l, [32, NT], F32, kind=dk).ap(); bWT = Buf()
            with ES() as ph:
                def tl(name, shp):
                    return P.sb(name, shp, F32, ph)
                gm = tl("Rgm", [128, 18]); geq = tl("Rgeq", [128, 18, 4]); gex = tl("Rgex", [128, 18, 4]); pg = tl("Rpg", [128, 18])
                elm = tl("Relm", [128, 18, 32]); m1 = tl("Rm1", [128, 18]); m2 = tl("Rm2", [128, 18]); s1 = tl("Rs1", [128, 18, 32]); s2 = tl("Rs2", [128, 18, 32])
                e2 = tl("Re2", [128, 18]); w1_ = tl("Rw1", [128, 18]); w2_ = tl("Rw2", [128, 18]); wts = tl("Rwts", [32, NT])
                bR = Buf()
                R_ = [bLG, bR, brw]
                ns = slice(t0, 18); nn = 18 - t0
                P.tt("dve", LG[:, ns], LG[:, ns], rb[:].unsqueeze(1).to_broadcast([128, nn, 36]), ALU.add, R_, [bLG])
                P.reduce(gm[:, ns], LG[:, ns, 0:4], ALU.max, R_, [bR])
                P.tt("dve", geq[:, ns], LG[:, ns, 0:4], gm[:, ns].unsqueeze(2).to_broadcast([128, nn, 4]), ALU.is_equal, R_, [bR])
                P.tt("dve", gex[:, ns], LG[:, ns, 0:4], gm[:, ns].unsqueeze(2).to_broadcast([128, nn, 4]), ALU.subtract, R_, [bR])
                P.act(gex[:, ns], gex[:, ns], AF.Exp, R_, [bR])
                P.reduce(pg[:, ns], gex[:, ns], ALU.add, R_, [bR])
                P.recip(pg[:, ns], pg[:, ns], R_, [bR])
                P.ts("dve", geq[:, ns], geq[:, ns], -1.0, 1e30, ALU.add, ALU.mult, reads=R_, writes=[bR])
                P.tt("dve", elm[:, ns].rearrange("p n (g e) -> p n g e", g=4), LG[:, ns, 4:36].rearrange("p n (g e) -> p n g e", g=4),
                     geq[:, ns].unsqueeze(3).to_broadcast([128, nn, 4, 8]), ALU.add, R_, [bR])
                P.reduce(m1[:, ns], elm[:, ns], ALU.max, R_, [bR])
                P.tt("dve", s1[:, ns], elm[:, ns], m1[:, ns].unsqueeze(2).to_broadcast([128, nn, 32]), ALU.is_equal, R_, [bR])
                P.stt(elm[:, ns], s1[:, ns], -1e30, elm[:, ns], ALU.mult, ALU.add, R_, [bR])
                P.reduce(m2[:, ns], elm[:, ns], ALU.max, R_, [bR])
                P.tt("dve", s2[:, ns], elm[:, ns], m2[:, ns].unsqueeze(2).to_broadcast([128, nn, 32]), ALU.is_equal, R_, [bR])
                P.tt("dve", e2[:, ns], m2[:, ns], m1[:, ns], ALU.subtract, R_, [bR])
                P.act(e2[:, ns], e2[:, ns], AF.Exp, R_, [bR])
                P.ts("dve", w1_[:, ns], e2[:, ns], 1.0, None, ALU.add, reads=R_, writes=[bR])
                P.recip(w1_[:, ns], w1_[:, ns], R_, [bR])
                P.tt("dve", w1_[:, ns], w1_[:, ns], pg[:, ns], ALU.mult, R_, [bR])
                P.tt("dve", w2_[:, ns], w1_[:, ns], e2[:, ns], ALU.mult, R_, [bR])
                P.tt("dve", s1[:, ns], s1[:, ns], w1_[:, ns].unsqueeze(2).to_broadcast([128, nn, 32]), ALU.mult, R_, [bR])
                P.tt("dve", s2[:, ns], s2[:, ns], w2_[:, ns].unsqueeze(2).to_broadcast([128, nn, 32]), ALU.mult, R_, [bR])
                P.tt("dve", WW[:, ns], s1[:, ns], s2[:, ns], ALU.add, R_, [bWW])
                ptw = P.ps("Rptw", [128, 512], F32, ph); bptw = Buf()
                for n in range(t0, 18):
                    P.tr(ptw[0:32, 0:128], WW[:, n, :], ident[:], [bWW, bconst], [bptw])
                    P.copy("dve", wts[:, n * 128:(n + 1) * 128], ptw[0:32, 0:128], [bptw], [bR])
                P.dma(WT_d[:, t0 * 128:NT], wts[:, t0 * 128:NT], reads=[bR], writes=[bWT])
                P.barrier()
            phx = ES(); phx.__enter__()
            xT = P.sb("ExT", [128, 8, NT], F32, phx); bx = Buf()
            with ES() as ph:
                hT = P.sb("EhT", [128, 8, NT], BF16, ph); bh = Buf()
                c_lo = qblocks[0][0]
                P.dma(hT[:, 0:4, c_lo:NT], HT_d[:, 0:4, c_lo:NT], reads=[bHT], writes=[bh]); P.dma(hT[:, 4:8, c_lo:NT], HT_d[:, 4:8, c_lo:NT], reads=[bHT], writes=[bh], q="pool")
                w1b = [P.sb("Ew1%d" % i, [128, 8, 512], BF16, ph) for i in range(2)]; w3b = [P.sb("Ew3%d" % i, [128, 8, 512], BF16, ph) for i in range(2)]
                w2b = P.sb("Ew2", [128, 4, 1024], BF16, ph)
                bw1 = [Buf(), Buf()]; bw3 = [Buf(), Buf()]; bw2 = Buf()
                stg = [P.sb("Estg%d" % i, [128, 2048], F32, ph) for i in range(2)]; bstg = [Buf(), Buf()]
                WB = [P.sb("EWB%d" % i, [128, 512], F32, ph) for i in range(2)]; bWB = [Buf(), Buf()]
                sgt = P.sb("Esg", [128, 512], F32, ph); t3 = P.sb("Et3", [128, 512], F32, ph); bsgt = Buf(); bt3 = Buf()
                gT = [P.sb("EgT%d" % i, [128, 4, 512], BF16, ph) for i in range(2)]; bgT = [Buf(), Buf()]
                ph1 = [P.ps("Eph1%d" % i, [128, 512], F32, ph) for i in range(2)]; bph1 = [Buf(), Buf()]
                ph3 = [P.ps("Eph3%d" % i, [128, 512], F32, ph) for i in range(2)]; bph3 = [Buf(), Buf()]
                pso = [P.ps("Epso%d" % i, [128, 512], F32, ph) for i in range(3)]; bpso = [Buf() for _ in range(3)]
                cnt = dict(ji=0, fi=0, oi=0)

                def loadw(src, dst, bdst, a_):
                    sv = src.rearrange("(k p) c -> p k c", p=128)
                    hk = a_ // 2
                    for half in range(2):
                        i2 = cnt["ji"] % 2; cnt["ji"] += 1
                        st_ = stg[i2][:].rearrange("p (a b) -> p a b", a=hk)
                        P.dma(st_, sv[:, half * hk:(half + 1) * hk, :], writes=[bstg[i2]], q="sp")
                        P.copy("act", dst[:, half * hk:(half + 1) * hk, :], st_, [bstg[i2]], [bdst])

                units = [(e, b0, bs) for e in range(32) for (b0, bs) in qblocks]
                nb = len(qblocks)

                def stage1(u):
                    e, b0, bs = units[u]
                    g_ = gT[u % 2]; bg_ = bgT[u % 2]
                    wb_ = WB[u % 2]; bwb_ = bWB[u % 2]
                    P.dma(wb_[:, :bs], WT_d[e, b0:b0 + bs].partition_broadcast(128), reads=[bWT], writes=[bwb_], q="pool")
                    for fc in range(4):
                        f2 = cnt["fi"] % 2; cnt["fi"] += 1
                        for kc in range(8):
                            P.mm(ph1[f2][:, :bs], w1b[e % 2][:, kc, fc * 128:(fc + 1) * 128], hT[:, kc, b0:b0 + bs], kc == 0, kc == 7, [bw1[e % 2], bh], [bph1[f2]])
                        for kc in range(8):
                            P.mm(ph3[f2][:, :bs], w3b[e % 2][:, kc, fc * 128:(fc + 1) * 128], hT[:, kc, b0:b0 + bs], kc == 0, kc == 7, [bw3[e % 2], bh], [bph3[f2]])
                        P.act(sgt[:, :bs], ph1[f2][:, :bs], AF.Silu, [bph1[f2]], [bsgt])
                        P.tt("dve", t3[:, :bs], ph3[f2][:, :bs], wb_[:, :bs], ALU.mult, [bph3[f2], bwb_], [bt3])
                        P.tt("pool", g_[:, fc, :bs], sgt[:, :bs], t3[:, :bs], ALU.mult, [bsgt, bt3], [bg_])

                def stage2(u):
                    e, b0, bs = units[u]
                    who = 1 if b0 < NCTX else 0
                    g_ = gT[u % 2]; bg_ = bgT[u % 2]
                    for oc in range(8):
                        o3 = cnt["oi"] % 3; cnt["oi"] += 1
                        for fc in range(4):
                            P.mm(pso[o3][:, :bs], w2b[:, fc, oc * 128:(oc + 1) * 128], g_[:, fc, :bs], fc == 0, fc == 3, [bw2, bg_], [bpso[o3]])
                        P.stt(xT[:, oc, b0:b0 + bs], pso[o3][:, :bs], MOD[:, 40 + oc, who:who + 1], xT[:, oc, b0:b0 + bs], ALU.mult, ALU.add, [bpso[o3], bMOD, bx], [bx])

                loadw(I["moe_w1"][l, 0], w1b[0], bw1[0], 8); loadw(I["moe_w3"][l, 0], w3b[0], bw3[0], 8); loadw(I["moe_w2"][l, 0], w2b, bw2, 4)
                for kc in range(8):
                    P.dma(xT[:, kc, c_lo:NT], XT_d[:, kc, c_lo:NT], reads=[bXT], writes=[bx], q="pool")
                for u in range(len(units) + 1):
                    if u < len(units):
                        e, b0, bs = units[u]
                        if u % nb == 0 and e + 1 < 32:
                            loadw(I["moe_w1"][l, e + 1], w1b[(e + 1) % 2], bw1[(e + 1) % 2], 8)
                            loadw(I["moe_w3"][l, e + 1], w3b[(e + 1) % 2], bw3[(e + 1) % 2], 8)
                        stage1(u)
                    if u >= 1:
                        stage2(u - 1)
                        e_, _, _ = units[u - 1]
                        if u % nb == 0 and e_ + 1 < 32:
                            loadw(I["moe_w2"][l, e_ + 1], w2b, bw2, 4)
                if not (last and layers == 2 and stop_after in (None, "ALL")):
                    for kc in range(8):
                        P.dma(XT_d[:, kc, c_lo:NT], xT[:, kc, c_lo:NT], reads=[bx], writes=[bXT], q="sp" if kc % 2 else "pool")
                P.barrier()
            if last and layers == 2 and stop_after in (None, "ALL"):
                final_phase(xT, bx)
            phx.__exit__(None, None, None)
        if stop_after == "E":
            P.emit(); return nc

    P.emit()
    return nc


_CACHE = {}


def make_in_maps(inputs, cores):
    consts = host_consts()
    maps = []
    for b in cores:
        m = {}
        for k, shp in IN_SHAPES.items():
            a = np.asarray(inputs[k], dtype=np.float32)
            if k in ("x", "ctx", "c"):
                a = a[b]
            m[k] = np.ascontiguousarray(a.reshape(shp))
        for k, v in consts.items():
            m["k_" + k] = np.ascontiguousarray(v)
        maps.append(m)
    return maps


def kernel(**inputs):
    if "nc" not in _CACHE:
        _CACHE["nc"] = build()
    nc = _CACHE["nc"]
    maps = make_in_maps(inputs, list(range(8)))
    res = run_bass_kernel_spmd(nc, maps, core_ids=list(range(8)))
    return np.stack([np.asarray(r["out"], dtype=np.float32) for r in res.results], axis=0)
```
